# Optimizing a Trainium2 kernel written in Bass

```python
import math
import jax
import jax.numpy as jnp
from jax import lax
import numpy as np

D_MODEL = 2048
BATCH = 8
SEQ = 4096
DEPTH = 1

HEAD_DIM = 128
D_MIX = D_MODEL
SB_HEADS = D_MIX // (2 * HEAD_DIM)
NSA_HEADS = D_MIX // (2 * HEAD_DIM)
NSA_KV_GROUPS = 2
NSA_GROUP_SIZE = NSA_HEADS // NSA_KV_GROUPS
SB_WIDTH = SB_HEADS * HEAD_DIM
NSA_WIDTH = NSA_HEADS * HEAD_DIM
NSA_KV_WIDTH = NSA_KV_GROUPS * HEAD_DIM
N_GATES = 3
CMP_STRIDE = 16
CMP_BLOCK = 2 * CMP_STRIDE
CMP_HIDDEN = 256
SEL_BLOCK = 64
SEL_TOP_N = 16
WINDOW = 512
SB_Q_BLOCK = 128
NSA_Q_BLOCK = 32
REL_BUCKETS = 32
REL_MAX_EXACT = 16
REL_MAX_DISTANCE = 1024
RMS_EPS = 1e-6
SEL_FORCE = 1e9
MASK_VALUE = -1e30
SPLIT_SIZES = (SB_WIDTH,) * 4 + (NSA_WIDTH,) + (NSA_KV_WIDTH,) * 6 + (N_GATES * NSA_HEADS, NSA_WIDTH)
D_IN_PROJ = sum(SPLIT_SIZES)

kernel_name = 'hybrid_stickbreaking_nsa_layer'


def rms_norm(x, g):
    xf = x.astype(jnp.float32)
    y = xf * lax.rsqrt(jnp.mean(xf * xf, axis=-1, keepdims=True) + RMS_EPS)
    return (y * g.astype(jnp.float32)).astype(x.dtype)


def rel_bucket(dist):
    n = jnp.maximum(dist, 0)
    nf = jnp.maximum(n, 1).astype(jnp.float32)
    large = REL_MAX_EXACT + (jnp.log(nf / REL_MAX_EXACT)
                             / math.log(REL_MAX_DISTANCE / REL_MAX_EXACT)
                             * (REL_BUCKETS - REL_MAX_EXACT)).astype(jnp.int32)
    large = jnp.minimum(large, REL_BUCKETS - 1)
    return jnp.where(n < REL_MAX_EXACT, n, large)


def masked_softmax(scores, mask):
    s = jnp.where(mask, scores.astype(jnp.float32), MASK_VALUE)
    return jnp.where(mask, jax.nn.softmax(s, axis=-1), 0.0)


def stick_breaking_attention(q, k, v):
    b, s, h, d = q.shape
    nqb = s // SB_Q_BLOCK
    q_blocks = q.reshape(b, nqb, SB_Q_BLOCK, h, d).transpose(1, 0, 2, 3, 4)
    vf = v.astype(jnp.float32)
    key_pos = jnp.arange(s)
    scale = 1.0 / math.sqrt(d)

    def block(args):
        q_blk, i = args
        t = i * SB_Q_BLOCK + jnp.arange(SB_Q_BLOCK)
        z = jnp.einsum('bthd,bshd->bhts', q_blk, k).astype(jnp.float32) * scale
        mask = key_pos[None, :] < t[:, None]
        log_beta = jax.nn.log_sigmoid(z)
        log_rest = jnp.where(mask, jax.nn.log_sigmoid(-z), 0.0)
        between = lax.cumsum(log_rest, axis=3, reverse=True) - log_rest
        a = jnp.where(mask, jnp.exp(log_beta + between), 0.0)
        return jnp.einsum('bhts,bshd->bthd', a, vf)

    out = lax.map(block, (q_blocks, jnp.arange(nqb)))
    return out.transpose(1, 0, 2, 3, 4).reshape(b, s, h * d).astype(q.dtype)


def compress_kv(kv, pos, w1, w2):
    b, s, g, d = kv.shape
    chunks = kv.reshape(b, s // CMP_STRIDE, CMP_STRIDE, g, d)
    blocks = jnp.concatenate([chunks[:, :-1], chunks[:, 1:]], axis=2)
    blocks = blocks + pos[None, None, :, None, :]
    hid = jax.nn.gelu(jnp.einsum('bnlgd,lde->bnge', blocks, w1))
    return jnp.einsum('bnge,ef->bngf', hid, w2)


def native_sparse_attention(q, k_cmp, v_cmp, k_sel, v_sel, k_win, v_win, gate_logits,
                            cmp_k_pos, cmp_k_w1, cmp_k_w2, cmp_v_pos, cmp_v_w1, cmp_v_w2,
                            rel_bias):
    b, s, g, r, d = q.shape
    scale = 1.0 / math.sqrt(d)
    kc = compress_kv(k_cmp, cmp_k_pos, cmp_k_w1, cmp_k_w2)
    vc = compress_kv(v_cmp, cmp_v_pos, cmp_v_w1, cmp_v_w2)
    nb = kc.shape[1]
    cmp_end = jnp.arange(nb) * CMP_STRIDE + CMP_BLOCK - 1
    nsel = s // SEL_BLOCK
    n_top = min(SEL_TOP_N, nsel)
    ci = np.arange(nb)[:, None] * CMP_STRIDE
    sj = np.arange(nsel)[None, :] * SEL_BLOCK
    overlap = jnp.asarray(((ci < sj + SEL_BLOCK) & (ci + CMP_BLOCK > sj)).astype(np.float32))
    ks_blocks = k_sel.reshape(b, nsel, SEL_BLOCK, g, d).transpose(0, 3, 1, 2, 4)
    vs_blocks = v_sel.reshape(b, nsel, SEL_BLOCK, g, d).transpose(0, 3, 1, 2, 4)
    kw_pad = jnp.pad(k_win, ((0, 0), (WINDOW, 0), (0, 0), (0, 0)))
    vw_pad = jnp.pad(v_win, ((0, 0), (WINDOW, 0), (0, 0), (0, 0)))
    win_len = NSA_Q_BLOCK + WINDOW
    bias_tbl = rel_bias.astype(jnp.float32).reshape(REL_BUCKETS, g, r).transpose(1, 2, 0)
    gather_blocks = jax.vmap(jax.vmap(lambda blk, idx: blk[idx]))

    nqb = s // NSA_Q_BLOCK
    q_blocks = q.reshape(b, nqb, NSA_Q_BLOCK, g, r, d).transpose(1, 0, 2, 3, 4, 5)
    g_blocks = gate_logits.reshape(b, nqb, NSA_Q_BLOCK, g, r, N_GATES).transpose(1, 0, 2, 3, 4, 5)

    def step(args):
        q_blk, g_blk, i = args
        start = i * NSA_Q_BLOCK
        t = start + jnp.arange(NSA_Q_BLOCK)
        bias_c = rel_bias[rel_bucket(t[:, None] - cmp_end[None, :])]
        bias_c = bias_c.transpose(2, 0, 1).reshape(g, r, NSA_Q_BLOCK, nb)
        sc = jnp.einsum('btgrd,bngd->bgrtn', q_blk, kc) * scale + bias_c
        p_c = masked_softmax(sc, cmp_end[None, :] <= t[:, None])
        o_c = jnp.einsum('bgrtn,bngd->btgrd', p_c, vc)
        imp = jnp.einsum('bgrtn,nj->bgtj', p_c, overlap)
        j = jnp.arange(nsel)[None, :]
        cur = (t // SEL_BLOCK)[:, None]
        valid = j * SEL_BLOCK <= t[:, None]
        forced = (j == 0) | (j == cur) | (j == cur - 1)
        score = jnp.where(valid, jnp.where(forced, SEL_FORCE, imp), -SEL_FORCE)
        _, idx = lax.top_k(score, n_top)
        ks_g = gather_blocks(ks_blocks, idx)
        vs_g = gather_blocks(vs_blocks, idx)
        key_pos = idx[..., None] * SEL_BLOCK + jnp.arange(SEL_BLOCK)
        dist = t[None, None, :, None, None] - key_pos
        bias_s = jnp.einsum('bgtnlk,grk->bgtrnl',
                            jax.nn.one_hot(rel_bucket(dist), REL_BUCKETS, dtype=jnp.float32), bias_tbl)
        ss = jnp.einsum('bgtrd,bgtnld->bgtrnl', q_blk.transpose(0, 2, 1, 3, 4), ks_g) * scale + bias_s
        n_keys = n_top * SEL_BLOCK
        p_s = masked_softmax(ss.reshape(b, g, NSA_Q_BLOCK, r, n_keys),
                             (dist >= 0).reshape(b, g, NSA_Q_BLOCK, 1, n_keys))
        o_s = jnp.einsum('bgtrm,bgtmd->btgrd', p_s, vs_g.reshape(b, g, NSA_Q_BLOCK, n_keys, d))
        kw = lax.dynamic_slice_in_dim(kw_pad, start, win_len, axis=1)
        vw = lax.dynamic_slice_in_dim(vw_pad, start, win_len, axis=1)
        win_pos = start - WINDOW + jnp.arange(win_len)
        diff = t[:, None] - win_pos[None, :]
        mask_w = (win_pos[None, :] >= 0) & (diff >= 0) & (diff < WINDOW)
        bias_w = rel_bias[rel_bucket(diff)].transpose(2, 0, 1).reshape(g, r, NSA_Q_BLOCK, win_len)
        sw = jnp.einsum('btgrd,bsgd->bgrts', q_blk, kw) * scale + bias_w
        p_w = masked_softmax(sw, mask_w)
        o_w = jnp.einsum('bgrts,bsgd->btgrd', p_w, vw)
        gate = jax.nn.sigmoid(g_blk.astype(jnp.float32))
        return gate[..., 0:1] * o_c + gate[..., 1:2] * o_s + gate[..., 2:3] * o_w

    out = lax.map(step, (q_blocks, g_blocks, jnp.arange(nqb)))
    return out.transpose(1, 0, 2, 3, 4, 5).reshape(b, s, g * r * d).astype(q.dtype)


def setup_inputs(seed: int = 0) -> dict:
    key = jax.random.key(seed)
    ks = jax.random.split(key, 16)
    f32 = jnp.float32
    nrm = lambda k, shape, sc: jax.random.normal(k, shape, f32) * sc
    return {
        'x': nrm(ks[0], (BATCH, SEQ, D_MODEL), 1.0),
        'norm_in': 1.0 + nrm(ks[1], (DEPTH, D_MODEL), 0.05),
        'w_in': nrm(ks[2], (DEPTH, D_MODEL, D_IN_PROJ), D_MODEL ** -0.5),
        'cmp_k_pos': nrm(ks[3], (DEPTH, CMP_BLOCK, HEAD_DIM), 0.5),
        'cmp_k_w1': nrm(ks[4], (DEPTH, CMP_BLOCK, HEAD_DIM, CMP_HIDDEN), (CMP_BLOCK * HEAD_DIM) ** -0.5),
        'cmp_k_w2': nrm(ks[5], (DEPTH, CMP_HIDDEN, HEAD_DIM), CMP_HIDDEN ** -0.5),
        'cmp_v_pos': nrm(ks[6], (DEPTH, CMP_BLOCK, HEAD_DIM), 0.5),
        'cmp_v_w1': nrm(ks[7], (DEPTH, CMP_BLOCK, HEAD_DIM, CMP_HIDDEN), (CMP_BLOCK * HEAD_DIM) ** -0.5),
        'cmp_v_w2': nrm(ks[8], (DEPTH, CMP_HIDDEN, HEAD_DIM), CMP_HIDDEN ** -0.5),
        'rel_bias': nrm(ks[9], (REL_BUCKETS, NSA_HEADS), 0.5),
        'norm_sb': 1.0 + nrm(ks[10], (DEPTH, SB_WIDTH), 0.05),
        'norm_nsa': 1.0 + nrm(ks[11], (DEPTH, NSA_WIDTH), 0.05),
        'w_out': nrm(ks[12], (DEPTH, D_MIX, D_MODEL), D_MIX ** -0.5),
        'norm_final': 1.0 + nrm(ks[13], (D_MODEL,), 0.05),
    }


def reference(x, norm_in, w_in, cmp_k_pos, cmp_k_w1, cmp_k_w2, cmp_v_pos, cmp_v_w1, cmp_v_w2,
              rel_bias, norm_sb, norm_nsa, w_out, norm_final):
    b, s, _ = x.shape
    split_points = [int(p) for p in np.cumsum(SPLIT_SIZES)[:-1]]
    g, r = NSA_KV_GROUPS, NSA_GROUP_SIZE
    h = x
    for layer in range(DEPTH):
        xn = rms_norm(h, norm_in[layer])
        proj = xn @ w_in[layer]
        (sb_q, sb_k, sb_v, sb_z, n_q, n_kc, n_vc, n_ks, n_vs, n_kw, n_vw,
         n_gate, n_z) = jnp.split(proj, split_points, axis=-1)
        sb_heads = lambda a: a.reshape(b, s, SB_HEADS, HEAD_DIM)
        kv_heads = lambda a: a.reshape(b, s, g, HEAD_DIM)
        o_sb = stick_breaking_attention(sb_heads(sb_q), sb_heads(sb_k), sb_heads(sb_v))
        o_nsa = native_sparse_attention(
            n_q.reshape(b, s, g, r, HEAD_DIM), kv_heads(n_kc), kv_heads(n_vc), kv_heads(n_ks),
            kv_heads(n_vs), kv_heads(n_kw), kv_heads(n_vw), n_gate.reshape(b, s, g, r, N_GATES),
            cmp_k_pos[layer], cmp_k_w1[layer], cmp_k_w2[layer],
            cmp_v_pos[layer], cmp_v_w1[layer], cmp_v_w2[layer], rel_bias)
        y_sb = rms_norm(o_sb, norm_sb[layer]) * jax.nn.silu(sb_z)
        y_nsa = rms_norm(o_nsa, norm_nsa[layer]) * jax.nn.silu(n_z)
        h = h + jnp.concatenate([y_sb, y_nsa], axis=-1) @ w_out[layer]
    return rms_norm(h, norm_final)
```

```python
import math
from contextlib import ExitStack

import numpy as np
import concourse.bass as bass
import concourse.mybir as mybir
from concourse.bass_utils import run_bass_kernel_spmd

F32 = mybir.dt.float32
BF16 = mybir.dt.bfloat16
AF = mybir.ActivationFunctionType
ALU = mybir.AluOpType
AX = mybir.AxisListType

S = 4096
D = 2048
NCOL = 7704
HD = 128
NEG = -30000.0
SCALE = 1.0 / math.sqrt(128.0)
EPS = 1e-6
C_SBQ, C_SBK, C_SBV, C_SBZ, C_NQ = 0, 1024, 2048, 3072, 4096
C_KC, C_VC, C_KS, C_VS, C_KW, C_VW, C_GATE, C_NZ = 5120, 5376, 5632, 5888, 6144, 6400, 6656, 6680
TM_SBV, TM_VS, TM_VW, TM_COLS = 0, 1024, 1280, 1536
LV_OFF = 4224
LV_LEN = 8448
WV_OFF = 512
WV_LEN = 2048


class Tile:
    __slots__ = ("w", "r")

    def __init__(self):
        self.w = None
        self.r = []


class Buf:
    def __init__(self, t):
        self.t = t
        self.tiles = {}

    def __getitem__(self, idx):
        return self.t[idx]

    def tl(self, key=0):
        tt = self.tiles.get(key)
        if tt is None:
            tt = self.tiles[key] = Tile()
        return tt


class View:
    def __init__(self, t, off, width):
        self.t = t
        self.off = off
        self.w = width
        self.tiles = {}

    def tl(self, key=0):
        tt = self.tiles.get(key)
        if tt is None:
            tt = self.tiles[key] = Tile()
        return tt

    def __getitem__(self, idx):
        r, c = idx
        a = 0 if c.start is None else c.start
        b = self.w if c.stop is None else c.stop
        return self.t[r, self.off + a:self.off + b]


class KB:
    ENGS = ("pe", "act", "dve", "pool", "sp")
    EPOCH = 12000

    def __init__(self, nc, stack):
        self.nc = nc
        self.stack = stack
        self.ops = {e: [] for e in self.ENGS}
        self.sems = {}
        self.semval = {}
        self.semh = {}
        self.waited = {e: {} for e in self.ENGS}
        self.nsem = 0
        for e in ("pe", "act", "dve", "pool"):
            self.sems[e] = self.newsem("c_" + e)
        self.nops = 0
        self.pe_keys = set([self.sems["pe"][0]])
        self.ninst = {e: 0 for e in self.ENGS}
        self.pending = {e: None for e in self.ENGS}
        self.pending_by_key = {}

    def newsem(self, name):
        self.nsem += 1
        s = self.stack.enter_context(self.nc.semaphore("%s_%d" % (name, self.nsem)))
        key = self.nsem
        self.semval[key] = 0
        self.semh[key] = s
        return (key, s)

    def _finalize(self, eng, force=False):
        rec = self.pending.get(eng)
        if rec is None:
            return
        if force:
            rec["sig"] = True
        if rec["sig"]:
            self.semval[rec["key"]] += 1
            assert self.semval[rec["key"]] == rec["val"]
        self.pending[eng] = None
        if self.pending_by_key.get(rec["key"]) is rec:
            del self.pending_by_key[rec["key"]]

    def op(self, eng, fn, reads=(), writes=(), dma_sem=None, extra=()):
        deps = list(extra)
        for t in reads:
            if t.w is not None:
                deps.append(t.w)
        for t in writes:
            if t.w is not None:
                deps.append(t.w)
            deps.extend(t.r)
        need = {}
        wd = self.waited[eng]
        pek = self.pe_keys if eng == "pe" else ()
        for (k, v) in deps:
            if k in pek or wd.get(k, 0) >= v:
                continue
            rec = self.pending_by_key.get(k)
            if rec is not None and rec["val"] == v:
                rec["sig"] = True
            if need.get(k, 0) < v:
                need[k] = v
        for k, v in need.items():
            wd[k] = v
        waits = [(self.semh[k], v) for k, v in need.items()]
        if dma_sem is not None:
            key, h = dma_sem
            self.semval[key] += 16
            tok = (key, self.semval[key])

            def run(e, fn=fn, waits=waits, h=h):
                for (wh, wv) in waits:
                    e.wait_ge(wh, wv)
                fn(e).then_inc(h, 16)
        else:
            key = self.sems[eng][0]
            self._finalize(eng, force=(self.semval[key] + 1 >= self.EPOCH))
            if self.semval[key] >= self.EPOCH:
                self.sems[eng] = self.newsem("c_" + eng)
                if eng == "pe":
                    self.pe_keys.add(self.sems[eng][0])
            key, h = self.sems[eng]
            rec = {"sig": eng != "pe", "val": self.semval[key] + 1, "key": key}
            self.pending[eng] = rec
            self.pending_by_key[key] = rec
            tok = (key, rec["val"])

            def run(e, fn=fn, waits=waits, h=h, rec=rec):
                for (wh, wv) in waits:
                    e.wait_ge(wh, wv)
                inst = fn(e)
                if rec["sig"]:
                    inst.then_inc(h, 1)

        self.ops[eng].append(run)
        self.ninst[eng] += 1
        for t in reads:
            t.r.append(tok)
        for t in writes:
            t.w = tok
            t.r = []
        return tok

    def barrier(self):
        for eng in ("pe", "act", "dve", "pool"):
            self._finalize(eng, force=True)
        allv = [(k, v) for k, v in self.semval.items() if v > 0]
        for eng in self.ENGS:
            wd = self.waited[eng]
            waits = []
            for (k, v) in allv:
                if wd.get(k, 0) < v:
                    waits.append((self.semh[k], v))
                    wd[k] = v

            def run(e, waits=waits):
                for (wh, wv) in waits:
                    e.wait_ge(wh, wv)

            self.ops[eng].append(run)
        self.emit()

    def emit(self):
        ops = self.ops
        with self.nc.Block() as blk:
            @blk.tensor
            def _(e):
                for f in ops["pe"]:
                    f(e)

            @blk.scalar
            def _(e):
                for f in ops["act"]:
                    f(e)

            @blk.vector
            def _(e):
                for f in ops["dve"]:
                    f(e)

            @blk.gpsimd
            def _(e):
                for f in ops["pool"]:
                    f(e)

            @blk.sync
            def _(e):
                for f in ops["sp"]:
                    f(e)
        self.ops = {e: [] for e in self.ENGS}


def _bucket(n):
    n = np.maximum(n, 0)
    nf = np.maximum(n, 1).astype(np.float32)
    large = 16 + (np.log(nf / np.float32(16)) / np.float32(math.log(1024 / 16)) * np.float32(16)).astype(np.int32)
    large = np.minimum(large, 31)
    return np.where(n < 16, n, large)


def _host_consts():
    c = {}
    dist = np.arange(LV_LEN) - LV_OFF
    oh = np.zeros((33, LV_LEN), np.float32)
    b = _bucket(dist)
    valid = dist >= 0
    oh[b[valid], np.nonzero(valid)[0]] = 1.0
    oh[32, ~valid] = 1.0
    c["oh_l"] = oh
    dist = np.arange(WV_LEN) - WV_OFF
    ohw = np.zeros((33, WV_LEN), np.float32)
    b = _bucket(dist)
    valid = (dist >= 0) & (dist < 512)
    ohw[b[valid], np.nonzero(valid)[0]] = 1.0
    ohw[32, ~valid] = 1.0
    c["oh_w"] = ohw
    t = np.arange(S)[:, None]
    j = np.arange(64)[None, :]
    cur = t // 64
    validb = (j * 64) <= t
    forced = (j == 0) | (j == cur) | (j == cur - 1)
    A = (validb & ~forced).astype(np.float32)
    B = np.where(validb, np.where(forced, 1e9, 0.0), -1e9).astype(np.float32)
    c["tk_a"] = np.ascontiguousarray(A.reshape(32, 128, 64).transpose(1, 0, 2))
    c["tk_b"] = np.ascontiguousarray(B.reshape(32, 128, 64).transpose(1, 0, 2))
    ci = np.arange(256)[:, None] * 16
    sj = np.arange(64)[None, :] * 64
    ov = ((ci < sj + 64) & (ci + 32 > sj)).astype(np.float32)
    ov[255, :] = 0.0
    c["overlap"] = np.ascontiguousarray(ov.reshape(2, 128, 64).transpose(1, 0, 2))
    X = (np.arange(S)[None, :] // 64 == np.arange(64)[:, None]).astype(np.float32)
    c["xexp"] = X * np.float32(30000.0)
    G = np.zeros((24, 24, 128), np.float32)
    for k in range(24):
        G[k, k, :] = 1.0
    c["gsel"] = G
    return c


def build(debug=0):
    nc = bass.Bass("TRN2", target_bir_lowering=False)
    dk = "ExternalOutput" if debug else "Internal"

    def din(name, shape, dt=F32):
        return nc.dram_tensor(name, list(shape), dt, kind="ExternalInput").ap()

    x_d = din("x", [S, D])
    w_in = din("w_in", [D, NCOL])
    w_out = din("w_out", [D, D])
    normin_d = din("norm_in", [1, D])
    normsb_d = din("norm_sb", [128, 8])
    normnsa_d = din("norm_nsa", [128, 8])
    normfin_d = din("norm_final", [1, D])
    relb_d = din("rel_bias", [32, 8])
    kw1_d = din("cmp_k_w1", [32, 128, 256])
    kw2_d = din("cmp_k_w2", [256, 128])
    kpos_d = din("cmp_k_pos", [32, 128])
    vw1_d = din("cmp_v_w1", [32, 128, 256])
    vw2_d = din("cmp_v_w2", [256, 128])
    vpos_d = din("cmp_v_pos", [32, 128])
    ohl_d = din("oh_l", [33, LV_LEN])
    ohw_d = din("oh_w", [33, WV_LEN])
    tka_d = din("tk_a", [128, 32, 64])
    tkb_d = din("tk_b", [128, 32, 64])
    ovl_d = din("overlap", [128, 2, 64])
    xexp_d = din("xexp", [64, S])
    gsel_d = din("gsel", [24, 24, 128])
    out_d = nc.dram_tensor("out", [S, D], F32, kind="ExternalOutput").ap()
    ft_d = nc.dram_tensor("ft", [NCOL, S], BF16, kind=dk).ap()
    tm_d = nc.dram_tensor("tm", [S, TM_COLS], BF16, kind=dk).ap()

    with ExitStack() as top:
        kb = KB(nc, top)

        uniq = [0]

        def sb(st, name, shape, dt):
            uniq[0] += 1
            return Buf(st.enter_context(nc.sbuf_tensor("s%d_%s" % (uniq[0], name), list(shape), dt)))

        def ps(st, name, shape, dt):
            uniq[0] += 1
            return Buf(st.enter_context(nc.psum_tensor("p%d_%s" % (uniq[0], name), list(shape), dt)))

        ident = sb(top, "ident", [128, 128], BF16)
        identf = sb(top, "identf", [128, 128], F32)
        negtri = sb(top, "negtri", [128, 128], BF16)
        negones = sb(top, "negones", [128, 128], BF16)
        ones = sb(top, "ones", [128, 128], BF16)
        kb.op("pool", lambda e: e.memset(ident[:], 0.0), writes=[ident.tl()])
        kb.op("pool", lambda e: e.affine_select(out=ident[:], in_=ident[:], pattern=[[-1, 128]],
                                                compare_op=ALU.not_equal, fill=1.0, base=0, channel_multiplier=1),
              reads=[ident.tl()], writes=[ident.tl()])
        kb.op("pool", lambda e: e.memset(identf[:], 0.0), writes=[identf.tl()])
        kb.op("pool", lambda e: e.affine_select(out=identf[:], in_=identf[:], pattern=[[-1, 128]],
                                                compare_op=ALU.not_equal, fill=1.0, base=0, channel_multiplier=1),
              reads=[identf.tl()], writes=[identf.tl()])
        antiI = sb(top, "antiI", [128, 128], BF16)
        kb.op("pool", lambda e: e.memset(antiI[:], 0.0), writes=[antiI.tl()])
        kb.op("pool", lambda e: e.affine_select(out=antiI[:], in_=antiI[:], pattern=[[1, 128]],
                                                compare_op=ALU.not_equal, fill=1.0, base=-127, channel_multiplier=1),
              reads=[antiI.tl()], writes=[antiI.tl()])
        kb.op("pool", lambda e: e.memset(negones[:], -1.0), writes=[negones.tl()])
        kb.op("pool", lambda e: e.memset(ones[:], 1.0), writes=[ones.tl()])
        kb.op("pool", lambda e: e.memset(negtri[:], -1.0), writes=[negtri.tl()])
        kb.op("pool", lambda e: e.affine_select(out=negtri[:], in_=negtri[:], pattern=[[-1, 128]],
                                                compare_op=ALU.is_ge, fill=0.0, base=0, channel_multiplier=1),
              reads=[negtri.tl()], writes=[negtri.tl()])

        with ExitStack() as st1:
            xnT = sb(st1, "xnT", [128, 16, S], BF16)
            with ExitStack() as sa:
                normt = sb(sa, "normt", [128, D], F32)
                xt = [sb(sa, "xt%d" % i, [128, D], F32) for i in range(2)]
                junk = sb(sa, "junk", [128, D], F32)
                xb = [sb(sa, "xb%d" % i, [128, D], BF16) for i in range(2)]
                ss = [sb(sa, "ss%d" % i, [128, 1], F32) for i in range(2)]
                rs = [sb(sa, "rs%d" % i, [128, 1], F32) for i in range(2)]
                ptr = [ps(sa, "ptr%d" % i, [128, 1024], BF16) for i in range(4)]
                s_x = [kb.newsem("s_x") for _ in range(2)]
                s_n = kb.newsem("s_n")
                kb.op("sp", lambda e: e.dma_start(out=normt[:], in_=normin_d.partition_broadcast(128)),
                      writes=[normt.tl()], dma_sem=s_n)
                for i in range(32):
                    b = i % 2
                    kb.op("sp", lambda e, i=i, b=b: e.dma_start(out=xt[b][:], in_=x_d[i * 128:(i + 1) * 128, :]),
                          writes=[xt[b].tl()], dma_sem=s_x[b])
                    kb.op("dve", lambda e, b=b: e.scalar_tensor_tensor(out=junk[:], in0=xt[b][:], scalar=1.0, in1=xt[b][:],
                                                                      op0=ALU.mult, op1=ALU.mult, accum_out=ss[b][:]),
                          reads=[xt[b].tl()], writes=[junk.tl(), ss[b].tl()])
                    kb.op("dve", lambda e, b=b: e.tensor_scalar(out=rs[b][:], in0=ss[b][:], scalar1=1.0 / D, scalar2=EPS,
                                                               op0=ALU.mult, op1=ALU.add),
                          reads=[ss[b].tl()], writes=[rs[b].tl()])
                    kb.op("act", lambda e, b=b: e.activation(out=rs[b][:], in_=rs[b][:], func=AF.Sqrt),
                          reads=[rs[b].tl()], writes=[rs[b].tl()])
                    kb.op("dve", lambda e, b=b: e.reciprocal(out=rs[b][:], in_=rs[b][:]),
                          reads=[rs[b].tl()], writes=[rs[b].tl()])
                    kb.op("dve", lambda e, b=b: e.scalar_tensor_tensor(out=xb[b][:], in0=xt[b][:], scalar=rs[b][:], in1=normt[:],
                                                                      op0=ALU.mult, op1=ALU.mult),
                          reads=[xt[b].tl(), rs[b].tl(), normt.tl()], writes=[xb[b].tl()])
                    for g in range(2):
                        pb = ptr[(2 * i + g) % 4]
                        for c in range(8):
                            cc = g * 8 + c
                            kb.op("pe", lambda e, b=b, pb=pb, c=c, cc=cc: e.transpose(
                                out=pb[:, c * 128:(c + 1) * 128], in_=xb[b][:, cc * 128:(cc + 1) * 128], identity=ident[:]),
                                reads=[xb[b].tl(), ident.tl()], writes=[pb.tl()])
                        dst = xnT[:, g * 8:(g + 1) * 8, i * 128:(i + 1) * 128]
                        src = pb[:].rearrange("p (c n) -> p c n", n=128)
                        if g == 0:
                            kb.op("act", lambda e, dst=dst, src=src: e.activation(out=dst, in_=src, func=AF.Copy),
                                  reads=[pb.tl()], writes=[xnT.tl(i)])
                        else:
                            kb.op("pool" if False else "dve", lambda e, dst=dst, src=src: e.tensor_copy(out=dst, in_=src),
                                  reads=[pb.tl()], writes=[xnT.tl(i)])
                kb.barrier()
                if debug == 10:
                    xnT_d = nc.dram_tensor("xnT_d", [128, 16, S], BF16, kind="ExternalOutput").ap()
                    s_dbg = kb.newsem("s_dbg")
                    kb.op("sp", lambda e: e.dma_start(out=xnT_d, in_=xnT[:]), dma_sem=s_dbg)
                    kb.barrier()
                    return nc
            with ExitStack() as sbk:
                NWB = 3
                wb = [sb(sbk, "wb%d" % i, [128, 16, 256], BF16) for i in range(NWB)]
                s_w = [kb.newsem("s_w") for _ in range(NWB)]
                stg = [sb(sbk, "stg%d" % i, [128, S], BF16) for i in range(2)]
                s_stg = [kb.newsem("s_stg") for _ in range(2)]
                stt = [sb(sbk, "stt%d" % i, [128, 8, 256], BF16) for i in range(2)]
                s_stt = [kb.newsem("s_stt") for _ in range(2)]
                pp = [ps(sbk, "pp%d" % i, [128, 512], F32) for i in range(6)]
                groups = []
                cidx = 0
                while cidx < C_GATE:
                    groups.append((cidx, 256))
                    cidx += 256
                groups.append((C_GATE, 24))
                cidx = C_NZ
                while cidx < NCOL:
                    groups.append((cidx, 256))
                    cidx += 256

                def col_kind(c0):
                    if C_SBV <= c0 < C_SBZ:
                        return ("tm", TM_SBV + c0 - C_SBV)
                    if C_VS <= c0 < C_KW:
                        return ("tm", TM_VS + c0 - C_VS)
                    if C_VW <= c0 < C_GATE:
                        return ("tm", TM_VW + c0 - C_VW)
                    if c0 < C_SBK or C_NQ <= c0 < C_KC:
                        return ("fm", "q")
                    if C_SBZ <= c0 < C_NQ or c0 >= C_NZ:
                        return ("fm", "silu")
                    if c0 == C_GATE:
                        return ("fm", "sig")
                    return ("fm", "copy")

                nfm = 0
                ntm = 0
                npp = 0
                import os
                if debug and os.environ.get("GSEL"):
                    groups = [groups[int(v)] for v in os.environ["GSEL"].split(",")]
                for gi, (c0, ncl) in enumerate(groups):
                    wbb = wb[gi % NWB]
                    kb.op("pool", lambda e, wbb=wbb, c0=c0, ncl=ncl: e.dma_start(
                        out=wbb[:, :, 0:ncl], in_=w_in[:, c0:c0 + ncl].rearrange("(c p) n -> p c n", p=128)),
                        writes=[wbb.tl()], dma_sem=s_w[gi % NWB])
                    kind, arg = col_kind(c0)
                    if kind == "fm":
                        for sgi in range((ncl + 127) // 128):
                            sc0 = sgi * 128
                            snc = min(128, ncl - sc0)
                            sg = stg[nfm % 2]
                            sgs = s_stg[nfm % 2]
                            nfm += 1
                            for qt in range(8):
                                pb = pp[npp % 6]
                                npp += 1
                                for kc in range(16):
                                    kb.op("pe", lambda e, pb=pb, wbb=wbb, kc=kc, sc0=sc0, snc=snc, qt=qt: e.matmul(
                                        pb[0:snc, :], lhsT=wbb[:, kc, sc0:sc0 + snc], rhs=xnT[:, kc, qt * 512:(qt + 1) * 512],
                                        start=(kc == 0), stop=(kc == 15)),
                                        reads=[wbb.tl()], writes=[pb.tl()])
                                dst = sg[0:snc, qt * 512:(qt + 1) * 512]
                                src = pb[0:snc, :]
                                if arg == "q":
                                    kb.op("act", lambda e, dst=dst, src=src: e.activation(out=dst, in_=src, func=AF.Copy, scale=SCALE),
                                          reads=[pb.tl()], writes=[sg.tl(qt)])
                                elif arg == "silu":
                                    kb.op("act", lambda e, dst=dst, src=src: e.activation(out=dst, in_=src, func=AF.Silu),
                                          reads=[pb.tl()], writes=[sg.tl(qt)])
                                elif arg == "sig":
                                    kb.op("act", lambda e, dst=dst, src=src: e.activation(out=dst, in_=src, func=AF.Sigmoid),
                                          reads=[pb.tl()], writes=[sg.tl(qt)])
                                else:
                                    kb.op("dve", lambda e, dst=dst, src=src: e.tensor_copy(out=dst, in_=src),
                                          reads=[pb.tl()], writes=[sg.tl(qt)])
                            kb.op("sp", lambda e, sg=sg, c0=c0, sc0=sc0, snc=snc: e.dma_start(
                                out=ft_d[c0 + sc0:c0 + sc0 + snc, :], in_=sg[0:snc, :]),
                                reads=[sg.tl(q) for q in range(8)], dma_sem=sgs)
                    else:
                        for i8 in range(4):
                            sg = stt[ntm % 2]
                            sgs = s_stt[ntm % 2]
                            ntm += 1
                            for ii in range(8):
                                i = i8 * 8 + ii
                                pb = pp[npp % 6]
                                npp += 1
                                for kc in range(16):
                                    kb.op("pe", lambda e, pb=pb, wbb=wbb, kc=kc, i=i, ncl=ncl: e.matmul(
                                        pb[:, 0:ncl], lhsT=xnT[:, kc, i * 128:(i + 1) * 128], rhs=wbb[:, kc, 0:ncl],
                                        start=(kc == 0), stop=(kc == 15)),
                                        reads=[wbb.tl()], writes=[pb.tl()])
                                dst = sg[:, ii, 0:ncl]
                                src = pb[:, 0:ncl]
                                if ii % 2 == 0:
                                    kb.op("dve", lambda e, dst=dst, src=src: e.tensor_copy(out=dst, in_=src),
                                          reads=[pb.tl()], writes=[sg.tl(ii)])
                                else:
                                    kb.op("act", lambda e, dst=dst, src=src: e.activation(out=dst, in_=src, func=AF.Copy),
                                          reads=[pb.tl()], writes=[sg.tl(ii)])
                            kb.op("sp", lambda e, sg=sg, arg=arg, i8=i8, ncl=ncl: e.dma_start(
                                out=tm_d[i8 * 1024:(i8 + 1) * 1024, arg:arg + ncl].rearrange("(i p) n -> p i n", p=128),
                                in_=sg[:, :, 0:ncl]),
                                reads=[sg.tl(q) for q in range(8)], dma_sem=sgs)
                kb.barrier()
        if debug == 1:
            return nc

        o_d = nc.dram_tensor("o_sc", [2048, S], BF16, kind=dk).ap()
        lv_d = nc.dram_tensor("lv_sc", [8, LV_LEN], BF16, kind=dk).ap()
        wv_d = nc.dram_tensor("wv_sc", [8, WV_LEN], BF16, kind=dk).ap()
        kcT = [sb(top, "kcT%d" % g, [128, 256], BF16) for g in range(2)]
        vcb = [sb(top, "vcb%d" % g, [128, 2, 128], BF16) for g in range(2)]

        def dma(eng, out, in_, sem, reads=(), writes=()):
            return kb.op(eng, lambda e: e.dma_start(out=out, in_=in_), reads=reads, writes=writes, dma_sem=sem)

        with ExitStack() as s2:
            relb = sb(s2, "relb", [33, 8], F32)
            ohl = sb(s2, "ohl", [33, LV_LEN], F32)
            ohw = sb(s2, "ohw", [33, WV_LEN], F32)
            lvs = sb(s2, "lvs", [8, LV_LEN], BF16)
            wvs = sb(s2, "wvs", [8, WV_LEN], BF16)
            pz = [ps(s2, "pz%d" % i, [128, 512], F32) for i in range(4)]
            sm = [kb.newsem("s2_%d" % i) for i in range(8)]
            kb.op("pool", lambda e: e.memset(relb[:], NEG), writes=[relb.tl()])
            dma("sp", relb[0:32, :], relb_d, sm[0], writes=[relb.tl()])
            dma("sp", ohl[:], ohl_d, sm[1], writes=[ohl.tl()])
            dma("sp", ohw[:], ohw_d, sm[2], writes=[ohw.tl()])
            nz = 0
            for (oh, n, dst, dd, sem) in ((ohl, LV_LEN, lvs, lv_d, sm[3]), (ohw, WV_LEN, wvs, wv_d, sm[4])):
                for c in range((n + 511) // 512):
                    w = min(512, n - c * 512)
                    pb = pz[nz % 4]
                    nz += 1
                    kb.op("pe", lambda e, pb=pb, oh=oh, c=c, w=w: e.matmul(pb[0:8, 0:w], lhsT=relb[:, :], rhs=oh[:, c * 512:c * 512 + w],
                                                                         start=True, stop=True),
                          reads=[relb.tl(), oh.tl()], writes=[pb.tl()])
                    kb.op("dve", lambda e, pb=pb, dst=dst, c=c, w=w: e.tensor_copy(out=dst[:, c * 512:c * 512 + w], in_=pb[0:8, 0:w]),
                          reads=[pb.tl()], writes=[dst.tl(c)])
                dma("sp", dd, dst[:], sem, reads=[dst.tl(c) for c in range((n + 511) // 512)])
            xcT = sb(s2, "xcT", [128, S], BF16)
            w1 = sb(s2, "w1", [128, 32, 256], BF16)
            w2 = sb(s2, "w2", [128, 2, 128], BF16)
            posf = sb(s2, "posf", [32, 128], F32)
            posT = sb(s2, "posT", [128, 32], BF16)
            hb = sb(s2, "hb", [128, 2], F32)
            hidT = sb(s2, "hidT", [128, 2, 256], BF16)
            g1 = sb(s2, "g1", [128, 256], F32)
            g2 = sb(s2, "g2", [128, 256], F32)
            g3 = sb(s2, "g3", [128, 256], F32)
            s_c = [kb.newsem("s2c_%d" % i) for i in range(4)]
            kb.op("pool", lambda e: e.memset(hidT[:], 0.0), writes=[hidT.tl(0), hidT.tl(1)])
            for which in range(2):
                w1_d, w2_d, pos_d, c_base = ((kw1_d, kw2_d, kpos_d, C_KC), (vw1_d, vw2_d, vpos_d, C_VC))[which]
                dma("pool", w1[:], w1_d.rearrange("l d e -> d l e"), s_c[0], writes=[w1.tl()])
                dma("pool", w2[:], w2_d.rearrange("(c p) f -> p c f", p=128), s_c[1], writes=[w2.tl()])
                dma("sp", posf[:], pos_d, s_c[2], writes=[posf.tl()])
                pb = pz[nz % 4]
                nz += 1
                kb.op("pe", lambda e, pb=pb: e.transpose(out=pb[:, 0:32], in_=posf[:, :], identity=identf[0:32, 0:32]),
                      reads=[posf.tl(), identf.tl()], writes=[pb.tl()])
                kb.op("dve", lambda e, pb=pb: e.tensor_copy(out=posT[:], in_=pb[:, 0:32]), reads=[pb.tl()], writes=[posT.tl()])
                pb = pz[nz % 4]
                nz += 1
                for eh in range(2):
                    for l in range(32):
                        kb.op("pe", lambda e, pb=pb, eh=eh, l=l: e.matmul(pb[:, eh:eh + 1], lhsT=w1[:, l, eh * 128:(eh + 1) * 128],
                                                                        rhs=posT[:, l:l + 1], start=(l == 0), stop=(l == 31)),
                              reads=[w1.tl(), posT.tl()], writes=[pb.tl()])
                kb.op("dve", lambda e, pb=pb: e.tensor_copy(out=hb[:], in_=pb[:, 0:2]), reads=[pb.tl()], writes=[hb.tl()])
                for g in range(2):
                    dma("sp", xcT[:], ft_d[c_base + g * 128:c_base + (g + 1) * 128, :], s_c[3], writes=[xcT.tl()])
                    for eh in range(2):
                        pb = pz[nz % 4]
                        nz += 1
                        for l in range(32):
                            kb.op("pe", lambda e, pb=pb, eh=eh, l=l: e.matmul(
                                pb[:, 0:255], lhsT=w1[:, l, eh * 128:(eh + 1) * 128],
                                rhs=xcT[:, l:l + 16 * 254 + 1:16], start=(l == 0), stop=(l == 31)),
                                reads=[w1.tl(), xcT.tl()], writes=[pb.tl()])
                        kb.op("act", lambda e, pb=pb, eh=eh: e.activation(out=g1[:, 0:255], in_=pb[:, 0:255], func=AF.Identity,
                                                                        bias=hb[:, eh:eh + 1]),
                              reads=[pb.tl(), hb.tl()], writes=[g1.tl()])
                        kb.op("dve", lambda e: e.tensor_tensor(out=g2[:, 0:255], in0=g1[:, 0:255], in1=g1[:, 0:255], op=ALU.mult),
                              reads=[g1.tl()], writes=[g2.tl()])
                        kb.op("dve", lambda e: e.tensor_scalar(out=g2[:, 0:255], in0=g2[:, 0:255], scalar1=0.044715, scalar2=1.0,
                                                               op0=ALU.mult, op1=ALU.add), reads=[g2.tl()], writes=[g2.tl()])
                        kb.op("dve", lambda e: e.tensor_tensor(out=g3[:, 0:255], in0=g2[:, 0:255], in1=g1[:, 0:255], op=ALU.mult),
                              reads=[g1.tl(), g2.tl()], writes=[g3.tl()])
                        kb.op("act", lambda e: e.activation(out=g3[:, 0:255], in_=g3[:, 0:255], func=AF.Sigmoid, scale=1.5957691216),
                              reads=[g3.tl()], writes=[g3.tl()])
                        kb.op("dve", lambda e, eh=eh: e.tensor_tensor(out=hidT[:, eh, 0:255], in0=g1[:, 0:255], in1=g3[:, 0:255], op=ALU.mult),
                              reads=[g1.tl(), g3.tl()], writes=[hidT.tl(eh)])
                    if which == 0:
                        pb = pz[nz % 4]
                        nz += 1
                        for eh in range(2):
                            kb.op("pe", lambda e, pb=pb, eh=eh: e.matmul(pb[:, 0:256], lhsT=w2[:, eh, :], rhs=hidT[:, eh, :],
                                                                       start=(eh == 0), stop=(eh == 1)),
                                  reads=[w2.tl(), hidT.tl(eh)], writes=[pb.tl()])
                        kb.op("dve", lambda e, pb=pb, g=g: e.tensor_copy(out=kcT[g][:], in_=pb[:, 0:256]), reads=[pb.tl()], writes=[kcT[g].tl()])
                    else:
                        for c in range(2):
                            pb = pz[nz % 4]
                            nz += 1
                            for eh in range(2):
                                kb.op("pe", lambda e, pb=pb, eh=eh, c=c: e.matmul(pb[:, 0:128], lhsT=hidT[:, eh, c * 128:(c + 1) * 128],
                                                                                rhs=w2[:, eh, :], start=(eh == 0), stop=(eh == 1)),
                                      reads=[w2.tl(), hidT.tl(eh)], writes=[pb.tl()])
                            kb.op("dve", lambda e, pb=pb, g=g, c=c: e.tensor_copy(out=vcb[g][:, c, :], in_=pb[:, 0:128]),
                                  reads=[pb.tl()], writes=[vcb[g].tl(c)])
            kb.barrier()
        if debug >= 2:
            dbg_d = nc.dram_tensor("dbg2", [128, 2, 256 + 256], BF16, kind="ExternalOutput").ap()
            sd = kb.newsem("sd")
            for g in range(2):
                dma("sp", dbg_d[:, g, 0:256], kcT[g][:], sd)
                dma("sp", dbg_d[:, g, 256:512], vcb[g][:].rearrange("p c f -> p (c f)"), sd)
            kb.barrier()
            if debug == 2:
                return nc

        def toep(tensor_ap, row, base, pstride, out_ap, sem, wt):
            src = bass.AP(tensor_ap.tensor, row * tensor_ap.shape[1] + base - 127 * pstride, [[pstride, 128], [1, 512]])
            return dma("sp", out_ap, src, sem, writes=wt)

        with ExitStack() as s3:
            PP = [ps(s3, "PP%d" % i, [128, 1024], F32) for i in range(4)]
            zb = [View(PP[0].t, 0, 512), View(PP[0].t, 512, 512), View(PP[1].t, 0, 512)]
            tbp = View(PP[1].t.bitcast(BF16), 1024, 1024)
            ub = View(PP[2].t, 0, 512)
            db = View(PP[2].t, 512, 512)
            mb = View(PP[3].t, 0, 512)
            gb = View(PP[3].t, 512, 512)
            nzb = [0]

            def nextz():
                b = zb[nzb[0] % 3]
                nzb[0] += 1
                return b

            with ExitStack() as ssb:
                qT = sb(ssb, "sb_qT", [128, S], BF16)
                kT = sb(ssb, "sb_kT", [128, S], BF16)
                vv = sb(ssb, "sb_v", [128, 32, 128], BF16)
                negm = sb(ssb, "negm", [128, 4, 512], BF16)
                ebuf = [sb(ssb, "ebuf%d" % i, [128, 1024], F32) for i in range(2)]
                spb = [sb(ssb, "spb%d" % i, [128, 1024], BF16) for i in range(4)]
                lsum = [sb(ssb, "lsum%d" % i, [128, 1024], BF16) for i in range(3)]
                aT = [sb(ssb, "aT%d" % i, [128, 1024], BF16) for i in range(3)]
                ost = [sb(ssb, "ost%d" % i, [128, 512], BF16) for i in range(2)]
                s_q, s_k, s_v = kb.newsem("sq"), kb.newsem("sk"), kb.newsem("sv")
                s_o = [kb.newsem("so") for _ in range(2)]
                kb.op("pool", lambda e: e.memset(negm[:], 0.0), writes=[negm.tl()])
                for di in range(4):
                    kb.op("pool", lambda e, di=di: e.affine_select(out=negm[:, di, :], in_=negm[:, di, :], pattern=[[1, 512]],
                                                                   compare_op=ALU.is_ge, fill=NEG, base=-128 * di - 1,
                                                                   channel_multiplier=-1),
                          reads=[negm.tl()], writes=[negm.tl()])
                ZZ = [View(PP[i].t, 0, 1024) for i in range(3)]
                ubs = [View(PP[3].t, 0, 512), View(PP[3].t, 512, 512)]
                nost = 0
                for h in range(8):
                    dma("sp", qT[:], ft_d[C_SBQ + h * 128:C_SBQ + (h + 1) * 128, :], s_q, writes=[qT.tl()])
                    dma("sp", kT[:], ft_d[C_SBK + h * 128:C_SBK + (h + 1) * 128, :], s_k, writes=[kT.tl()])
                    dma("sp", vv[:], tm_d[:, TM_SBV + h * 128:TM_SBV + (h + 1) * 128].rearrange("(i p) d -> p i d", p=128), s_v,
                        writes=[vv.tl()])
                    tasks = []
                    for qt in range(8):
                        kbs = list(range(4 * qt + 3, -1, -1))
                        for j in range(len(kbs) // 2):
                            tasks.append((qt, j, kbs[2 * j], kbs[2 * j + 1], len(kbs)))
                    nt = len(tasks)
                    st = {}

                    def stA(ti):
                        qt, j, kba, kbb, n = tasks[ti]
                        Z = ZZ[ti % 3]
                        for half, kbi in ((0, kba), (1, kbb)):
                            c0 = half * 512
                            kb.op("pe", lambda e, Z=Z, kbi=kbi, qt=qt, c0=c0: e.matmul(
                                Z[:, c0:c0 + 512], lhsT=kT[:, kbi * 128:(kbi + 1) * 128], rhs=qT[:, qt * 512:(qt + 1) * 512],
                                start=True, stop=False), reads=[kT.tl(), qT.tl()], writes=[Z.tl()])
                            if kbi >= 4 * qt:
                                di = kbi - 4 * qt
                                kb.op("pe", lambda e, Z=Z, di=di, c0=c0: e.matmul(Z[:, c0:c0 + 512], lhsT=ident[:, :], rhs=negm[:, di, :],
                                                                                start=False, stop=False),
                                      reads=[ident.tl(), negm.tl()], writes=[Z.tl()])
                        eb = ebuf[ti % 2]
                        sp = spb[ti % 4]
                        kb.op("act", lambda e, Z=Z, eb=eb: e.activation(out=eb[:], in_=Z[:, 0:1024], func=AF.Exp),
                              reads=[Z.tl()], writes=[eb.tl()])
                        kb.op("act", lambda e, eb=eb, sp=sp: e.activation(out=sp[:], in_=eb[:], func=AF.Ln, bias=1.0),
                              reads=[eb.tl()], writes=[sp.tl()])
                        ls = lsum[ti % 3]
                        if j == 0:
                            kb.op("pool", lambda e, sp=sp, ls=ls: e.tensor_copy(out=ls[:, 512:1024], in_=sp[:, 0:512]),
                                  reads=[sp.tl()], writes=[ls.tl()])
                        else:
                            psp, pls = st[ti - 1][1], st[ti - 1][2]
                            kb.op("pool", lambda e, psp=psp, pls=pls, ls=ls: e.tensor_tensor(
                                out=ls[:, 0:512], in0=pls[:, 512:1024], in1=psp[:, 512:1024], op=ALU.add),
                                reads=[psp.tl(), pls.tl()], writes=[ls.tl()])
                            kb.op("pool", lambda e, sp=sp, ls=ls: e.tensor_tensor(
                                out=ls[:, 512:1024], in0=ls[:, 0:512], in1=sp[:, 0:512], op=ALU.add),
                                reads=[sp.tl(), ls.tl()], writes=[ls.tl()])
                        st[ti] = (Z, sp, ls)

                    def stB1(ti):
                        qt, j, kba, kbb, n = tasks[ti]
                        Z, sp, ls = st[ti]
                        for half in (0, 1):
                            c0 = half * 512
                            idx = 2 * j + half
                            kb.op("pe", lambda e, Z=Z, sp=sp, c0=c0, idx=idx: e.matmul(Z[:, c0:c0 + 512], lhsT=negtri[:, :], rhs=sp[:, c0:c0 + 512],
                                                                                     start=False, stop=(idx == 0)),
                                  reads=[negtri.tl(), sp.tl()], writes=[Z.tl()])
                            if idx > 0:
                                kb.op("pe", lambda e, Z=Z, ls=ls, c0=c0: e.matmul(Z[:, c0:c0 + 512], lhsT=negones[:, :], rhs=ls[:, c0:c0 + 512],
                                                                                start=False, stop=True),
                                      reads=[negones.tl(), ls.tl()], writes=[Z.tl()])
                        a_ = aT[ti % 3]
                        kb.op("act", lambda e, Z=Z, a_=a_: e.activation(out=a_[:], in_=Z[:, 0:1024], func=AF.Exp),
                              reads=[Z.tl()], writes=[a_.tl()])

                    def stB2(ti):
                        nonlocal nost
                        qt, j, kba, kbb, n = tasks[ti]
                        a_ = aT[ti % 3]
                        u = ubs[qt % 2]
                        for half, kbi in ((0, kba), (1, kbb)):
                            c0 = half * 512
                            idx = 2 * j + half
                            kb.op("pe", lambda e, a_=a_, kbi=kbi, idx=idx, n=n, u=u, c0=c0: e.matmul(
                                u[:, :], lhsT=vv[:, kbi, :], rhs=a_[:, c0:c0 + 512], start=(idx == 0), stop=(idx == n - 1)),
                                reads=[vv.tl(), a_.tl()], writes=[u.tl()])
                        if 2 * j + 2 == n:
                            o = ost[nost % 2]
                            so = s_o[nost % 2]
                            nost += 1
                            kb.op("dve", lambda e, o=o, u=u: e.tensor_copy(out=o[:], in_=u[:, :]), reads=[u.tl()], writes=[o.tl()])
                            dma("sp", o_d[h * 128:(h + 1) * 128, qt * 512:(qt + 1) * 512], o[:], so, reads=[o.tl()])

                    for step in range(nt + 2):
                        if step < nt:
                            stA(step)
                        if 0 <= step - 1 < nt:
                            stB1(step - 1)
                        if 0 <= step - 2 < nt:
                            stB2(step - 2)
                kb.barrier()
            if debug == 3:
                return nc

            with ExitStack() as sn:
                qTn = [sb(sn, "n_qT%d" % r, [128, S], BF16) for r in range(4)]
                ocb = [sb(sn, "ocb%d" % r, [128, S], BF16) for r in range(4)]
                gateT = sb(sn, "gateT", [24, S], BF16)
                gsel = sb(sn, "gsel", [24, 24, 128], BF16)
                selT = sb(sn, "selT", [64, S], BF16)
                pT = [sb(sn, "pT%d" % i, [128, 512], BF16) for i in range(3)]
                rd = sb(sn, "rd", [128, 512], F32)
                ff = sb(sn, "ff", [128, 512], F32)
                tmpf = sb(sn, "tmpf", [128, 512], F32)
                oacc = sb(sn, "oacc", [128, 512], F32)
                ostn = [sb(sn, "ostn%d" % i, [128, 512], BF16) for i in range(2)]
                s_on = [kb.newsem("son") for _ in range(2)]
                s_ld = [kb.newsem("snl") for _ in range(12)]
                s_cb = [kb.newsem("scb") for _ in range(12)]
                s_sb = [kb.newsem("ssb") for _ in range(14)]
                s_wb = [kb.newsem("swb") for _ in range(8)]
                dma("sp", gateT[:], ft_d[C_GATE:C_GATE + 24, :], s_ld[0], writes=[gateT.tl()])
                dma("pool", gsel[:], gsel_d, s_ld[1], writes=[gsel.tl()])
                relb31 = sb(sn, "relb31", [128, 8], F32)
                dma("sp", relb31[:], relb_d[31:32, :].partition_broadcast(128), s_ld[11], writes=[relb31.tl()])
                npt = [0]
                nos = [0]

                def nextp():
                    p = pT[npt[0] % 3]
                    npt[0] += 1
                    return p

                def finalize(gidx, qt):
                    kb.op("dve", lambda e: e.tensor_scalar(out=rd[:], in0=db[:, :], scalar1=1e-30, scalar2=None, op0=ALU.max),
                          reads=[db.tl()], writes=[rd.tl()])
                    kb.op("dve", lambda e: e.reciprocal(out=rd[:], in_=rd[:]), reads=[rd.tl()], writes=[rd.tl()])
                    kb.op("pe", lambda e: e.matmul(gb[:, :], lhsT=gsel[:, gidx, :], rhs=gateT[:, qt * 512:(qt + 1) * 512],
                                                   start=True, stop=True),
                          reads=[gsel.tl(), gateT.tl()], writes=[gb.tl()])
                    kb.op("dve", lambda e: e.tensor_tensor(out=ff[:], in0=rd[:], in1=gb[:, :], op=ALU.mult),
                          reads=[rd.tl(), gb.tl()], writes=[ff.tl()])

                for g in range(2):
                    for r in range(4):
                        h = g * 4 + r
                        dma("sp", qTn[r][:], ft_d[C_NQ + h * 128:C_NQ + (h + 1) * 128, :], s_ld[2 + r], writes=[qTn[r].tl()])
                    with ExitStack() as p1:
                        impT = sb(p1, "impT", [64, S], F32)
                        tka = sb(p1, "tka", [128, 32, 64], F32)
                        tkb = sb(p1, "tkb", [128, 32, 64], F32)
                        ovl = sb(p1, "ovl", [128, 2, 64], BF16)
                        cbias = sb(p1, "cbias", [128, 12, 512], BF16)
                        sc = sb(p1, "sc", [128, 64], F32)
                        sc2 = sb(p1, "sc2", [128, 64], F32)
                        m8a = sb(p1, "m8a", [128, 8], F32)
                        m8b = sb(p1, "m8b", [128, 8], F32)
                        snb = sb(p1, "snb", [128, 64], BF16)
                        dma("sp", tka[:], tka_d, s_ld[6], writes=[tka.tl()])
                        dma("sp", tkb[:], tkb_d, s_ld[7], writes=[tkb.tl()])
                        dma("pool", ovl[:], ovl_d, s_ld[8], writes=[ovl.tl()])
                        pairs = [(qt, 0) for qt in range(8)] + [(qt, 1) for qt in range(4, 8)]
                        pidx = {pr: i for i, pr in enumerate(pairs)}
                        for r in range(4):
                            h = g * 4 + r
                            for ci, (qt, c) in enumerate(pairs):
                                toep(lv_d, h, LV_OFF + 512 * qt - 2048 * c - 31, 16, cbias[:, ci, :], s_cb[ci], [cbias.tl(ci)])
                            for qt in range(8):
                                cs = [0] if qt < 4 else [0, 1]
                                for k_, c in enumerate(cs):
                                    z = nextz()
                                    ci = pidx[(qt, c)]
                                    kb.op("pe", lambda e, z=z, c=c, r=r, qt=qt: e.matmul(
                                        z[:, :], lhsT=kcT[g][:, c * 128:(c + 1) * 128], rhs=qTn[r][:, qt * 512:(qt + 1) * 512],
                                        start=True, stop=False), reads=[kcT[g].tl(), qTn[r].tl()], writes=[z.tl()])
                                    kb.op("pe", lambda e, z=z, ci=ci: e.matmul(z[:, :], lhsT=antiI[:, :], rhs=cbias[:, ci, :],
                                                                             start=False, stop=True),
                                          reads=[antiI.tl(), cbias.tl(ci)], writes=[z.tl()])
                                    p = nextp()
                                    kb.op("act", lambda e, z=z, p=p: e.activation(out=p[:], in_=z[:, :], func=AF.Exp),
                                          reads=[z.tl()], writes=[p.tl()])
                                    first, last = (k_ == 0), (k_ == len(cs) - 1)
                                    kb.op("pe", lambda e, p=p, c=c, first=first, last=last: e.matmul(
                                        ub[:, :], lhsT=vcb[g][:, c, :], rhs=p[:], start=first, stop=last),
                                        reads=[vcb[g].tl(c), p.tl()], writes=[ub.tl()])
                                    kb.op("pe", lambda e, p=p, first=first, last=last: e.matmul(
                                        db[:, :], lhsT=ones[:, :], rhs=p[:], start=first, stop=last),
                                        reads=[ones.tl(), p.tl()], writes=[db.tl()])
                                    kb.op("pe", lambda e, p=p, c=c, first=first, last=last: e.matmul(
                                        mb[0:64, :], lhsT=ovl[:, c, :], rhs=p[:], start=first, stop=last),
                                        reads=[ovl.tl(), p.tl()], writes=[mb.tl()])
                                finalize(g * 12 + r * 3 + 0, qt)
                                kb.op("dve", lambda e, r=r, qt=qt: e.tensor_tensor(out=ocb[r][:, qt * 512:(qt + 1) * 512], in0=ub[:, :],
                                                                                  in1=ff[:], op=ALU.mult),
                                      reads=[ub.tl(), ff.tl()], writes=[ocb[r].tl(qt)])
                                if r == 0:
                                    kb.op("dve", lambda e, qt=qt: e.tensor_tensor(out=impT[:, qt * 512:(qt + 1) * 512], in0=mb[0:64, :],
                                                                                 in1=rd[0:64, :], op=ALU.mult),
                                          reads=[mb.tl(), rd.tl()], writes=[impT.tl(qt)])
                                else:
                                    kb.op("dve", lambda e: e.tensor_tensor(out=tmpf[0:64, :], in0=mb[0:64, :], in1=rd[0:64, :], op=ALU.mult),
                                          reads=[mb.tl(), rd.tl()], writes=[tmpf.tl()])
                                    kb.op("dve", lambda e, qt=qt: e.tensor_tensor(out=impT[:, qt * 512:(qt + 1) * 512],
                                                                                 in0=impT[:, qt * 512:(qt + 1) * 512], in1=tmpf[0:64, :],
                                                                                 op=ALU.add),
                                          reads=[tmpf.tl(), impT.tl(qt)], writes=[impT.tl(qt)])
                        for i in range(32):
                            kb.op("pe", lambda e, i=i: e.transpose(out=gb[:, 0:64], in_=impT[:, i * 128:(i + 1) * 128],
                                                                   identity=identf[0:64, 0:64]),
                                  reads=[impT.tl(i // 4), identf.tl()], writes=[gb.tl()])
                            kb.op("dve", lambda e, i=i: e.tensor_tensor(out=sc[:], in0=gb[:, 0:64], in1=tka[:, i, :], op=ALU.mult),
                                  reads=[gb.tl(), tka.tl()], writes=[sc.tl()])
                            kb.op("dve", lambda e, i=i: e.tensor_tensor(out=sc[:], in0=sc[:], in1=tkb[:, i, :], op=ALU.add),
                                  reads=[sc.tl(), tkb.tl()], writes=[sc.tl()])
                            kb.op("dve", lambda e: e.max(out=m8a[:], in_=sc[:]), reads=[sc.tl()], writes=[m8a.tl()])
                            kb.op("dve", lambda e: e.match_replace(out=sc2[:], in_to_replace=m8a[:], in_values=sc[:], imm_value=-3e9),
                                  reads=[sc.tl(), m8a.tl()], writes=[sc2.tl()])
                            kb.op("dve", lambda e: e.max(out=m8b[:], in_=sc2[:]), reads=[sc2.tl()], writes=[m8b.tl()])
                            kb.op("dve", lambda e: e.tensor_scalar(out=snb[:], in0=sc[:], scalar1=m8b[:, 7:8], scalar2=-1.0,
                                                                   op0=ALU.is_ge, op1=ALU.add),
                                  reads=[sc.tl(), m8b.tl()], writes=[snb.tl()])
                            kb.op("pe", lambda e, i=i: e.transpose(out=tbp[0:64, (i % 8) * 128:(i % 8 + 1) * 128], in_=snb[:, :],
                                                                   identity=ident[:, :]),
                                  reads=[snb.tl(), ident.tl()], writes=[tbp.tl()])
                            if i % 8 == 7:
                                i8 = i // 8
                                kb.op("dve", lambda e, i8=i8: e.tensor_copy(out=selT[:, i8 * 1024:(i8 + 1) * 1024], in_=tbp[0:64, :]),
                                      reads=[tbp.tl()], writes=[selT.tl(2 * i8), selT.tl(2 * i8 + 1)])
                        kb.barrier()
                    with ExitStack() as p2:
                        ksT = sb(p2, "ksT", [128, S], BF16)
                        kwT = sb(p2, "kwT", [128, S], BF16)
                        vs = sb(p2, "vs", [128, 32, 128], BF16)
                        vw = sb(p2, "vw", [128, 32, 128], BF16)
                        x30 = sb(p2, "x30", [64, S], BF16)
                        sbias = sb(p2, "sbias", [128, 14, 512], BF16)
                        wbias = sb(p2, "wbias", [128, 8, 512], BF16)
                        dma("sp", ksT[:], ft_d[C_KS + g * 128:C_KS + (g + 1) * 128, :], s_ld[6], writes=[ksT.tl()])
                        dma("sp", kwT[:], ft_d[C_KW + g * 128:C_KW + (g + 1) * 128, :], s_ld[7], writes=[kwT.tl()])
                        dma("sp", vs[:], tm_d[:, TM_VS + g * 128:TM_VS + (g + 1) * 128].rearrange("(i p) d -> p i d", p=128), s_ld[8],
                            writes=[vs.tl()])
                        dma("sp", vw[:], tm_d[:, TM_VW + g * 128:TM_VW + (g + 1) * 128].rearrange("(i p) d -> p i d", p=128), s_ld[9],
                            writes=[vw.tl()])
                        dma("pool", x30[:], xexp_d, s_ld[10], writes=[x30.tl()])
                        for r in range(4):
                            h = g * 4 + r
                            for ti in range(14):
                                toep(lv_d, h, LV_OFF + (-384 + 128 * ti), 1, sbias[:, ti, :], s_sb[ti], [sbias.tl(ti)])
                            for ti in range(8):
                                toep(wv_d, h, WV_OFF + (-384 + 128 * ti), 1, wbias[:, ti, :], s_wb[ti], [wbias.tl(ti)])
                            tasks = []
                            for qt in range(8):
                                kbs = list(range(0, 4 * qt + 4))
                                for idx, kbi in enumerate(kbs):
                                    tasks.append(("s", qt, idx, kbi, len(kbs)))
                                kbs = list(range(max(0, 4 * qt - 4), 4 * qt + 4))
                                for idx, kbi in enumerate(kbs):
                                    tasks.append(("w", qt, idx, kbi, len(kbs)))
                            stt_ = {}

                            def front(ti, r=r, h=h):
                                far = False
                                kind, qt, idx, kbi, n = tasks[ti]
                                z = nextz()
                                delta = 512 * qt - 128 * kbi
                                if kind == "s":
                                    tix = min((delta + 384) // 128, 13)
                                    kb.op("pe", lambda e, z=z, kbi=kbi, qt=qt: e.matmul(
                                        z[:, :], lhsT=ksT[:, kbi * 128:(kbi + 1) * 128], rhs=qTn[r][:, qt * 512:(qt + 1) * 512],
                                        start=True, stop=False), reads=[ksT.tl(), qTn[r].tl()], writes=[z.tl()])
                                    far = delta >= 1280
                                    if not far:
                                        kb.op("pe", lambda e, z=z, tix=tix: e.matmul(z[:, :], lhsT=antiI[:, :], rhs=sbias[:, tix, :],
                                                                                   start=False, stop=False),
                                              reads=[antiI.tl(), sbias.tl(tix)], writes=[z.tl()])
                                    kb.op("pe", lambda e, z=z, kbi=kbi, qt=qt: e.matmul(
                                        z[:, :], lhsT=x30[:, kbi * 128:(kbi + 1) * 128], rhs=selT[:, qt * 512:(qt + 1) * 512],
                                        start=False, stop=True), reads=[x30.tl(), selT.tl(qt)], writes=[z.tl()])
                                else:
                                    tix = (delta + 384) // 128
                                    kb.op("pe", lambda e, z=z, kbi=kbi, qt=qt: e.matmul(
                                        z[:, :], lhsT=kwT[:, kbi * 128:(kbi + 1) * 128], rhs=qTn[r][:, qt * 512:(qt + 1) * 512],
                                        start=True, stop=False), reads=[kwT.tl(), qTn[r].tl()], writes=[z.tl()])
                                    kb.op("pe", lambda e, z=z, tix=tix: e.matmul(z[:, :], lhsT=antiI[:, :], rhs=wbias[:, tix, :],
                                                                               start=False, stop=True),
                                          reads=[antiI.tl(), wbias.tl(tix)], writes=[z.tl()])
                                p = nextp()
                                if kind == "s" and far:
                                    kb.op("act", lambda e, z=z, p=p: e.activation(out=p[:], in_=z[:, :], func=AF.Exp,
                                                                                 bias=relb31[:, h:h + 1]),
                                          reads=[z.tl(), relb31.tl()], writes=[p.tl()])
                                else:
                                    kb.op("act", lambda e, z=z, p=p: e.activation(out=p[:], in_=z[:, :], func=AF.Exp),
                                          reads=[z.tl()], writes=[p.tl()])
                                stt_[ti] = p

                            def back(ti, r=r, g=g, h=h):
                                kind, qt, idx, kbi, n = tasks[ti]
                                p = stt_.pop(ti)
                                first, last = (idx == 0), (idx == n - 1)
                                u = ub if kind == "s" else mb
                                vsrc = vs if kind == "s" else vw
                                kb.op("pe", lambda e, p=p, kbi=kbi, first=first, last=last, u=u, vsrc=vsrc: e.matmul(
                                    u[:, :], lhsT=vsrc[:, kbi, :], rhs=p[:], start=first, stop=last),
                                    reads=[vsrc.tl(), p.tl()], writes=[u.tl()])
                                kb.op("pe", lambda e, p=p, first=first, last=last: e.matmul(
                                    db[:, :], lhsT=ones[:, :], rhs=p[:], start=first, stop=last),
                                    reads=[ones.tl(), p.tl()], writes=[db.tl()])
                                if not last:
                                    return
                                if kind == "s":
                                    finalize(g * 12 + r * 3 + 1, qt)
                                    kb.op("dve", lambda e: e.tensor_tensor(out=oacc[:], in0=ub[:, :], in1=ff[:], op=ALU.mult),
                                          reads=[ub.tl(), ff.tl()], writes=[oacc.tl()])
                                    kb.op("dve", lambda e, qt=qt: e.tensor_tensor(out=oacc[:], in0=oacc[:],
                                                                                 in1=ocb[r][:, qt * 512:(qt + 1) * 512], op=ALU.add),
                                          reads=[oacc.tl(), ocb[r].tl(qt)], writes=[oacc.tl()])
                                else:
                                    finalize(g * 12 + r * 3 + 2, qt)
                                    kb.op("dve", lambda e: e.tensor_tensor(out=tmpf[:], in0=mb[:, :], in1=ff[:], op=ALU.mult),
                                          reads=[mb.tl(), ff.tl()], writes=[tmpf.tl()])
                                    o = ostn[nos[0] % 2]
                                    so = s_on[nos[0] % 2]
                                    nos[0] += 1
                                    kb.op("dve", lambda e, o=o: e.tensor_tensor(out=o[:], in0=oacc[:], in1=tmpf[:], op=ALU.add),
                                          reads=[oacc.tl(), tmpf.tl()], writes=[o.tl()])
                                    dma("sp", o_d[1024 + h * 128:1024 + (h + 1) * 128, qt * 512:(qt + 1) * 512], o[:], so, reads=[o.tl()])

                            nt = len(tasks)
                            front(0)
                            for ti in range(nt):
                                if ti + 1 < nt:
                                    front(ti + 1)
                                back(ti)
                        kb.barrier()
            if debug == 4:
                return nc

        with ExitStack() as s4:
            wo = sb(s4, "wo", [128, 16, D], BF16)
            nf = sb(s4, "nf", [128, D], F32)
            gn = sb(s4, "gn", [128, 16], F32)
            ot = sb(s4, "ot", [128, 16, 512], BF16)
            szt = sb(s4, "szt", [128, 16, 512], BF16)
            yTs = [sb(s4, "yT%d" % i, [128, 16, 512], BF16) for i in range(2)]
            sq = [sb(s4, "sq%d" % i, [128, 512], BF16) for i in range(2)]
            rstdb = [sb(s4, "rstdb%d" % i, [128, 512], F32) for i in range(2)]
            tmp4 = sb(s4, "tmp4", [128, 512], F32)
            xt2 = [sb(s4, "xt2_%d" % i, [128, D], F32) for i in range(2)]
            hb2 = sb(s4, "hb2", [128, D], F32)
            junk2 = sb(s4, "junk2", [128, D], F32)
            ob = sb(s4, "ob", [128, D], F32)
            ss2 = sb(s4, "ss2", [128, 1], F32)
            rs2 = sb(s4, "rs2", [128, 1], F32)
            pss = [ps(s4, "pss%d" % i, [128, 512], F32) for i in range(2)]
            ppo = [ps(s4, "ppo%d" % i, [128, 512], F32) for i in range(4)]
            s_w4 = [kb.newsem("sw4") for _ in range(4)]
            s_m = [kb.newsem("s4m") for _ in range(6)]
            s_x2 = [kb.newsem("sx2") for _ in range(2)]
            s_out = kb.newsem("sout")
            wo_v = w_out.rearrange("(c p) n -> p c n", p=128)
            for i in range(4):
                dma("pool", wo[:, 4 * i:4 * i + 4, :], wo_v[:, 4 * i:4 * i + 4, :], s_w4[i], writes=[wo.tl(i)])
            dma("sp", nf[:], normfin_d.partition_broadcast(128), s_m[0], writes=[nf.tl()])
            dma("sp", gn[:, 0:8], normsb_d, s_m[1], writes=[gn.tl(0)])
            dma("sp", gn[:, 8:16], normnsa_d, s_m[2], writes=[gn.tl(1)])
            npo = 0
            nx = 0

            def prep_parts(qt):
                yTq = yTs[qt % 2]

                def load():
                    dma("act", ot[:], o_d[:, qt * 512:(qt + 1) * 512].rearrange("(h p) t -> p h t", p=128), s_m[3], writes=[ot.tl()])
                    dma("act", szt[:, 0:8, :], ft_d[C_SBZ:C_SBZ + 1024, qt * 512:(qt + 1) * 512].rearrange("(h p) t -> p h t", p=128),
                        s_m[4], writes=[szt.tl(0)])
                    dma("act", szt[:, 8:16, :], ft_d[C_NZ:C_NZ + 1024, qt * 512:(qt + 1) * 512].rearrange("(h p) t -> p h t", p=128),
                        s_m[5], writes=[szt.tl(1)])

                def stat1(G, hh):
                    i = G * 8 + hh
                    q_ = sq[i % 2]
                    kb.op("act", lambda e, q_=q_, i=i: e.activation(out=q_[:], in_=ot[:, i, :], func=AF.Square),
                          reads=[ot.tl()], writes=[q_.tl()])
                    kb.op("pe", lambda e, q_=q_, G=G, hh=hh: e.matmul(pss[G][:, :], lhsT=ones[:, :], rhs=q_[:], start=(hh == 0), stop=(hh == 7)),
                          reads=[ones.tl(), q_.tl()], writes=[pss[G].tl()])

                def rstd(G):
                    rb_ = rstdb[G]
                    kb.op("dve", lambda e, rb_=rb_, G=G: e.tensor_scalar(out=rb_[:], in0=pss[G][:, :], scalar1=1.0 / 1024, scalar2=EPS,
                                                                         op0=ALU.mult, op1=ALU.add),
                          reads=[pss[G].tl()], writes=[rb_.tl()])
                    kb.op("act", lambda e, rb_=rb_: e.activation(out=rb_[:], in_=rb_[:], func=AF.Sqrt), reads=[rb_.tl()], writes=[rb_.tl()])
                    kb.op("dve", lambda e, rb_=rb_: e.reciprocal(out=rb_[:], in_=rb_[:]), reads=[rb_.tl()], writes=[rb_.tl()])

                def y1(G, hh):
                    rb_ = rstdb[G]
                    i = G * 8 + hh
                    kb.op("dve", lambda e, i=i, rb_=rb_: e.scalar_tensor_tensor(out=tmp4[:], in0=ot[:, i, :], scalar=gn[:, i:i + 1],
                                                                               in1=rb_[:], op0=ALU.mult, op1=ALU.mult),
                          reads=[ot.tl(), gn.tl(G), rb_.tl()], writes=[tmp4.tl()])
                    kb.op("dve", lambda e, i=i: e.tensor_tensor(out=yTq[:, i, :], in0=tmp4[:], in1=szt[:, i, :], op=ALU.mult),
                          reads=[tmp4.tl(), szt.tl(G)], writes=[yTq.tl(i)])

                th = [load]
                for G in range(2):
                    for hh in range(8):
                        th.append(lambda G=G, hh=hh: stat1(G, hh))
                    th.append(lambda G=G: rstd(G))
                for G in range(2):
                    for hh in range(8):
                        th.append(lambda G=G, hh=hh: y1(G, hh))
                return th

            def proj_tile(qt, ti):
                nonlocal npo, nx
                yTq = yTs[qt % 2]
                tok0 = qt * 512 + ti * 128
                xb_ = xt2[nx % 2]
                sx_ = s_x2[nx % 2]
                nx += 1
                dma("sp", xb_[:], x_d[tok0:tok0 + 128, :], sx_, writes=[xb_.tl()])
                for nb in range(4):
                    pb = ppo[npo % 4]
                    npo += 1
                    for f in range(16):
                        kb.op("pe", lambda e, pb=pb, f=f, nb=nb: e.matmul(
                            pb[:, :], lhsT=yTq[:, f, ti * 128:(ti + 1) * 128], rhs=wo[:, f, nb * 512:(nb + 1) * 512],
                            start=(f == 0), stop=(f == 15)), reads=[yTq.tl(f), wo.tl(f // 4)], writes=[pb.tl()])
                    kb.op("dve", lambda e, pb=pb, nb=nb, xb_=xb_: e.tensor_tensor(out=hb2[:, nb * 512:(nb + 1) * 512], in0=pb[:, :],
                                                                                 in1=xb_[:, nb * 512:(nb + 1) * 512], op=ALU.add),
                          reads=[pb.tl(), xb_.tl()], writes=[hb2.tl(nb)])
                    for _ in range(3):
                        if prepq:
                            prepq.pop(0)()
                hts = [hb2.tl(nb) for nb in range(4)]
                kb.op("act", lambda e: e.activation(out=junk2[:], in_=hb2[:], func=AF.Square, accum_out=ss2[:]),
                      reads=hts, writes=[junk2.tl(), ss2.tl()])
                kb.op("dve", lambda e: e.tensor_scalar(out=rs2[:], in0=ss2[:], scalar1=1.0 / D, scalar2=EPS, op0=ALU.mult, op1=ALU.add),
                      reads=[ss2.tl()], writes=[rs2.tl()])
                kb.op("act", lambda e: e.activation(out=rs2[:], in_=rs2[:], func=AF.Sqrt), reads=[rs2.tl()], writes=[rs2.tl()])
                kb.op("dve", lambda e: e.reciprocal(out=rs2[:], in_=rs2[:]), reads=[rs2.tl()], writes=[rs2.tl()])
                kb.op("act", lambda e: e.activation(out=junk2[:], in_=hb2[:], func=AF.Copy, scale=rs2[:, 0:1]),
                      reads=hts + [rs2.tl()], writes=[junk2.tl()])
                kb.op("pool", lambda e: e.tensor_tensor(out=ob[:], in0=junk2[:], in1=nf[:], op=ALU.mult),
                      reads=[junk2.tl(), nf.tl()], writes=[ob.tl()])
                dma("sp", out_d[tok0:tok0 + 128, :], ob[:], s_out, reads=[ob.tl()])

            prepq = []
            for f_ in prep_parts(0):
                f_()
            for qt in range(8):
                if qt + 1 < 8:
                    prepq.extend(prep_parts(qt + 1))
                    prepq.pop(0)()
                for ti in range(4):
                    proj_tile(qt, ti)
                while prepq:
                    prepq.pop(0)()
            kb.barrier()
    return nc


_NC_CACHE = {}


def _prep_inputs(inputs):
    c = _host_consts()
    f = lambda a: np.ascontiguousarray(np.asarray(a, dtype=np.float32))
    shared = {
        "w_in": f(inputs["w_in"][0]),
        "w_out": f(inputs["w_out"][0]),
        "norm_in": f(inputs["norm_in"][0]).reshape(1, D),
        "norm_sb": f(np.asarray(inputs["norm_sb"][0]).reshape(8, 128).T),
        "norm_nsa": f(np.asarray(inputs["norm_nsa"][0]).reshape(8, 128).T),
        "norm_final": f(inputs["norm_final"]).reshape(1, D),
        "rel_bias": f(inputs["rel_bias"]),
        "cmp_k_w1": f(inputs["cmp_k_w1"][0]),
        "cmp_k_w2": f(inputs["cmp_k_w2"][0]),
        "cmp_k_pos": f(inputs["cmp_k_pos"][0]),
        "cmp_v_w1": f(inputs["cmp_v_w1"][0]),
        "cmp_v_w2": f(inputs["cmp_v_w2"][0]),
        "cmp_v_pos": f(inputs["cmp_v_pos"][0]),
    }
    shared.update(c)
    return shared


def kernel(**inputs):
    shared = _prep_inputs(inputs)
    x = np.asarray(inputs["x"], dtype=np.float32)
    if "nc" not in _NC_CACHE:
        _NC_CACHE["nc"] = build(0)
    nc = _NC_CACHE["nc"]
    in_maps = []
    for b in range(8):
        m = dict(shared)
        m["x"] = np.ascontiguousarray(x[b])
        in_maps.append(m)
    res = run_bass_kernel_spmd(nc, in_maps, core_ids=list(range(8)))
    out = np.stack([np.asarray(res.results[b]["out"]) for b in range(8)], axis=0)
    return out.astype(np.float32)
```

```python
import math
from contextlib import ExitStack

import numpy as np
import concourse.bass as bass
import concourse.mybir as mybir
from concourse.bass_utils import run_bass_kernel_spmd

F32 = mybir.dt.float32
BF16 = mybir.dt.bfloat16
AF = mybir.ActivationFunctionType
ALU = mybir.AluOpType
AX = mybir.AxisListType

S = 4096
D = 2048
NCOL = 7704
HD = 128
NEG = -30000.0
SCALE = 1.0 / math.sqrt(128.0)
EPS = 1e-6
C_SBQ, C_SBK, C_SBV, C_SBZ, C_NQ = 0, 1024, 2048, 3072, 4096
C_KC, C_VC, C_KS, C_VS, C_KW, C_VW, C_GATE, C_NZ = 5120, 5376, 5632, 5888, 6144, 6400, 6656, 6680
TM_SBV, TM_VS, TM_VW, TM_COLS = 0, 1024, 1280, 1536
LV_OFF = 4224
LV_LEN = 8448
WV_OFF = 512
WV_LEN = 2048


class Tile:
    __slots__ = ("w", "r")

    def __init__(self):
        self.w = None
        self.r = []


class Buf:
    def __init__(self, t):
        self.t = t
        self.tiles = {}

    def __getitem__(self, idx):
        return self.t[idx]

    def tl(self, key=0):
        tt = self.tiles.get(key)
        if tt is None:
            tt = self.tiles[key] = Tile()
        return tt


class View:
    def __init__(self, t, off, width):
        self.t = t
        self.off = off
        self.w = width
        self.tiles = {}

    def tl(self, key=0):
        tt = self.tiles.get(key)
        if tt is None:
            tt = self.tiles[key] = Tile()
        return tt

    def __getitem__(self, idx):
        r, c = idx
        a = 0 if c.start is None else c.start
        b = self.w if c.stop is None else c.stop
        return self.t[r, self.off + a:self.off + b]


class KB:
    ENGS = ("pe", "act", "dve", "pool", "sp")
    EPOCH = 12000

    def __init__(self, nc, stack):
        self.nc = nc
        self.stack = stack
        self.ops = {e: [] for e in self.ENGS}
        self.sems = {}
        self.semval = {}
        self.semh = {}
        self.waited = {e: {} for e in self.ENGS}
        self.nsem = 0
        for e in ("pe", "act", "dve", "pool"):
            self.sems[e] = self.newsem("c_" + e)
        self.nops = 0
        self.pe_keys = set([self.sems["pe"][0]])
        self.ninst = {e: 0 for e in self.ENGS}
        self.pending = {e: None for e in self.ENGS}
        self.pending_by_key = {}

    def newsem(self, name):
        self.nsem += 1
        s = self.stack.enter_context(self.nc.semaphore("%s_%d" % (name, self.nsem)))
        key = self.nsem
        self.semval[key] = 0
        self.semh[key] = s
        return (key, s)

    def _finalize(self, eng, force=False):
        rec = self.pending.get(eng)
        if rec is None:
            return
        if force:
            rec["sig"] = True
        if rec["sig"]:
            self.semval[rec["key"]] += 1
            assert self.semval[rec["key"]] == rec["val"]
        self.pending[eng] = None
        if self.pending_by_key.get(rec["key"]) is rec:
            del self.pending_by_key[rec["key"]]

    def op(self, eng, fn, reads=(), writes=(), dma_sem=None, extra=()):
        deps = list(extra)
        for t in reads:
            if t.w is not None:
                deps.append(t.w)
        for t in writes:
            if t.w is not None:
                deps.append(t.w)
            deps.extend(t.r)
        need = {}
        wd = self.waited[eng]
        pek = self.pe_keys if eng == "pe" else ()
        for (k, v) in deps:
            if k in pek or wd.get(k, 0) >= v:
                continue
            rec = self.pending_by_key.get(k)
            if rec is not None and rec["val"] == v:
                rec["sig"] = True
            if need.get(k, 0) < v:
                need[k] = v
        for k, v in need.items():
            wd[k] = v
        waits = [(self.semh[k], v) for k, v in need.items()]
        if dma_sem is not None:
            key, h = dma_sem
            self.semval[key] += 16
            tok = (key, self.semval[key])

            def run(e, fn=fn, waits=waits, h=h):
                for (wh, wv) in waits:
                    e.wait_ge(wh, wv)
                fn(e).then_inc(h, 16)
        else:
            key = self.sems[eng][0]
            self._finalize(eng, force=(self.semval[key] + 1 >= self.EPOCH))
            if self.semval[key] >= self.EPOCH:
                self.sems[eng] = self.newsem("c_" + eng)
                if eng == "pe":
                    self.pe_keys.add(self.sems[eng][0])
            key, h = self.sems[eng]
            rec = {"sig": eng != "pe", "val": self.semval[key] + 1, "key": key}
            self.pending[eng] = rec
            self.pending_by_key[key] = rec
            tok = (key, rec["val"])

            def run(e, fn=fn, waits=waits, h=h, rec=rec):
                for (wh, wv) in waits:
                    e.wait_ge(wh, wv)
                inst = fn(e)
                if rec["sig"]:
                    inst.then_inc(h, 1)

        self.ops[eng].append(run)
        self.ninst[eng] += 1
        for t in reads:
            t.r.append(tok)
        for t in writes:
            t.w = tok
            t.r = []
        return tok

    def barrier(self):
        for eng in ("pe", "act", "dve", "pool"):
            self._finalize(eng, force=True)
        allv = [(k, v) for k, v in self.semval.items() if v > 0]
        for eng in self.ENGS:
            wd = self.waited[eng]
            waits = []
            for (k, v) in allv:
                if wd.get(k, 0) < v:
                    waits.append((self.semh[k], v))
                    wd[k] = v

            def run(e, waits=waits):
                for (wh, wv) in waits:
                    e.wait_ge(wh, wv)

            self.ops[eng].append(run)
        self.emit()

    def emit(self):
        ops = self.ops
        with self.nc.Block() as blk:
            @blk.tensor
            def _(e):
                for f in ops["pe"]:
                    f(e)

            @blk.scalar
            def _(e):
                for f in ops["act"]:
                    f(e)

            @blk.vector
            def _(e):
                for f in ops["dve"]:
                    f(e)

            @blk.gpsimd
            def _(e):
                for f in ops["pool"]:
                    f(e)

            @blk.sync
            def _(e):
                for f in ops["sp"]:
                    f(e)
        self.ops = {e: [] for e in self.ENGS}


def _bucket(n):
    n = np.maximum(n, 0)
    nf = np.maximum(n, 1).astype(np.float32)
    large = 16 + (np.log(nf / np.float32(16)) / np.float32(math.log(1024 / 16)) * np.float32(16)).astype(np.int32)
    large = np.minimum(large, 31)
    return np.where(n < 16, n, large)


def _host_consts():
    c = {}
    dist = np.arange(LV_LEN) - LV_OFF
    oh = np.zeros((33, LV_LEN), np.float32)
    b = _bucket(dist)
    valid = dist >= 0
    oh[b[valid], np.nonzero(valid)[0]] = 1.0
    oh[32, ~valid] = 1.0
    c["oh_l"] = oh
    dist = np.arange(WV_LEN) - WV_OFF
    ohw = np.zeros((33, WV_LEN), np.float32)
    b = _bucket(dist)
    valid = (dist >= 0) & (dist < 512)
    ohw[b[valid], np.nonzero(valid)[0]] = 1.0
    ohw[32, ~valid] = 1.0
    c["oh_w"] = ohw
    t = np.arange(S)[:, None]
    j = np.arange(64)[None, :]
    cur = t // 64
    validb = (j * 64) <= t
    forced = (j == 0) | (j == cur) | (j == cur - 1)
    A = (validb & ~forced).astype(np.float32)
    B = np.where(validb, np.where(forced, 1e9, 0.0), -1e9).astype(np.float32)
    c["tk_a"] = np.ascontiguousarray(A.reshape(32, 128, 64).transpose(1, 0, 2))
    c["tk_b"] = np.ascontiguousarray(B.reshape(32, 128, 64).transpose(1, 0, 2))
    ci = np.arange(256)[:, None] * 16
    sj = np.arange(64)[None, :] * 64
    ov = ((ci < sj + 64) & (ci + 32 > sj)).astype(np.float32)
    ov[255, :] = 0.0
    c["overlap"] = np.ascontiguousarray(ov.reshape(2, 128, 64).transpose(1, 0, 2))
    X = (np.arange(S)[None, :] // 64 == np.arange(64)[:, None]).astype(np.float32)
    c["xexp"] = X * np.float32(30000.0)
    G = np.zeros((24, 24, 128), np.float32)
    for k in range(24):
        G[k, k, :] = 1.0
    c["gsel"] = G
    return c


def build(debug=0):
    nc = bass.Bass("TRN2", target_bir_lowering=False)
    dk = "ExternalOutput" if debug else "Internal"

    def din(name, shape, dt=F32):
        return nc.dram_tensor(name, list(shape), dt, kind="ExternalInput").ap()

    x_d = din("x", [S, D])
    w_in = din("w_in", [D, NCOL])
    w_out = din("w_out", [D, D])
    normin_d = din("norm_in", [1, D])
    normsb_d = din("norm_sb", [128, 8])
    normnsa_d = din("norm_nsa", [128, 8])
    normfin_d = din("norm_final", [1, D])
    relb_d = din("rel_bias", [32, 8])
    kw1_d = din("cmp_k_w1", [32, 128, 256])
    kw2_d = din("cmp_k_w2", [256, 128])
    kpos_d = din("cmp_k_pos", [32, 128])
    vw1_d = din("cmp_v_w1", [32, 128, 256])
    vw2_d = din("cmp_v_w2", [256, 128])
    vpos_d = din("cmp_v_pos", [32, 128])
    ohl_d = din("oh_l", [33, LV_LEN])
    ohw_d = din("oh_w", [33, WV_LEN])
    tka_d = din("tk_a", [128, 32, 64])
    tkb_d = din("tk_b", [128, 32, 64])
    ovl_d = din("overlap", [128, 2, 64])
    xexp_d = din("xexp", [64, S])
    gsel_d = din("gsel", [24, 24, 128])
    out_d = nc.dram_tensor("out", [S, D], F32, kind="ExternalOutput").ap()
    ft_d = nc.dram_tensor("ft", [NCOL, S], BF16, kind=dk).ap()
    tm_d = nc.dram_tensor("tm", [S, TM_COLS], BF16, kind=dk).ap()

    with ExitStack() as top:
        kb = KB(nc, top)

        uniq = [0]

        def sb(st, name, shape, dt):
            uniq[0] += 1
            return Buf(st.enter_context(nc.sbuf_tensor("s%d_%s" % (uniq[0], name), list(shape), dt)))

        def ps(st, name, shape, dt):
            uniq[0] += 1
            return Buf(st.enter_context(nc.psum_tensor("p%d_%s" % (uniq[0], name), list(shape), dt)))

        ident = sb(top, "ident", [128, 128], BF16)
        identf = sb(top, "identf", [128, 128], F32)
        negtri = sb(top, "negtri", [128, 128], BF16)
        negones = sb(top, "negones", [128, 128], BF16)
        ones = sb(top, "ones", [128, 128], BF16)
        kb.op("pool", lambda e: e.memset(ident[:], 0.0), writes=[ident.tl()])
        kb.op("pool", lambda e: e.affine_select(out=ident[:], in_=ident[:], pattern=[[-1, 128]],
                                                compare_op=ALU.not_equal, fill=1.0, base=0, channel_multiplier=1),
              reads=[ident.tl()], writes=[ident.tl()])
        kb.op("pool", lambda e: e.memset(identf[:], 0.0), writes=[identf.tl()])
        kb.op("pool", lambda e: e.affine_select(out=identf[:], in_=identf[:], pattern=[[-1, 128]],
                                                compare_op=ALU.not_equal, fill=1.0, base=0, channel_multiplier=1),
              reads=[identf.tl()], writes=[identf.tl()])
        antiI = sb(top, "antiI", [128, 128], BF16)
        kb.op("pool", lambda e: e.memset(antiI[:], 0.0), writes=[antiI.tl()])
        kb.op("pool", lambda e: e.affine_select(out=antiI[:], in_=antiI[:], pattern=[[1, 128]],
                                                compare_op=ALU.not_equal, fill=1.0, base=-127, channel_multiplier=1),
              reads=[antiI.tl()], writes=[antiI.tl()])
        kb.op("pool", lambda e: e.memset(negones[:], -1.0), writes=[negones.tl()])
        kb.op("pool", lambda e: e.memset(ones[:], 1.0), writes=[ones.tl()])
        kb.op("pool", lambda e: e.memset(negtri[:], -1.0), writes=[negtri.tl()])
        kb.op("pool", lambda e: e.affine_select(out=negtri[:], in_=negtri[:], pattern=[[-1, 128]],
                                                compare_op=ALU.is_ge, fill=0.0, base=0, channel_multiplier=1),
              reads=[negtri.tl()], writes=[negtri.tl()])

        with ExitStack() as st1:
            xnT = sb(st1, "xnT", [128, 16, S], BF16)
            with ExitStack() as sa:
                normt = sb(sa, "normt", [128, D], F32)
                xt = [sb(sa, "xt%d" % i, [128, D], F32) for i in range(2)]
                junk = sb(sa, "junk", [128, D], F32)
                xb = [sb(sa, "xb%d" % i, [128, D], BF16) for i in range(2)]
                ss = [sb(sa, "ss%d" % i, [128, 1], F32) for i in range(2)]
                rs = [sb(sa, "rs%d" % i, [128, 1], F32) for i in range(2)]
                ptr = [ps(sa, "ptr%d" % i, [128, 1024], BF16) for i in range(4)]
                s_x = [kb.newsem("s_x") for _ in range(2)]
                s_n = kb.newsem("s_n")
                kb.op("sp", lambda e: e.dma_start(out=normt[:], in_=normin_d.partition_broadcast(128)),
                      writes=[normt.tl()], dma_sem=s_n)
                for i in range(32):
                    b = i % 2
                    kb.op("sp", lambda e, i=i, b=b: e.dma_start(out=xt[b][:], in_=x_d[i * 128:(i + 1) * 128, :]),
                          writes=[xt[b].tl()], dma_sem=s_x[b])
                    kb.op("dve", lambda e, b=b: e.scalar_tensor_tensor(out=junk[:], in0=xt[b][:], scalar=1.0, in1=xt[b][:],
                                                                      op0=ALU.mult, op1=ALU.mult, accum_out=ss[b][:]),
                          reads=[xt[b].tl()], writes=[junk.tl(), ss[b].tl()])
                    kb.op("dve", lambda e, b=b: e.tensor_scalar(out=rs[b][:], in0=ss[b][:], scalar1=1.0 / D, scalar2=EPS,
                                                               op0=ALU.mult, op1=ALU.add),
                          reads=[ss[b].tl()], writes=[rs[b].tl()])
                    kb.op("act", lambda e, b=b: e.activation(out=rs[b][:], in_=rs[b][:], func=AF.Sqrt),
                          reads=[rs[b].tl()], writes=[rs[b].tl()])
                    kb.op("dve", lambda e, b=b: e.reciprocal(out=rs[b][:], in_=rs[b][:]),
                          reads=[rs[b].tl()], writes=[rs[b].tl()])
                    kb.op("dve", lambda e, b=b: e.scalar_tensor_tensor(out=xb[b][:], in0=xt[b][:], scalar=rs[b][:], in1=normt[:],
                                                                      op0=ALU.mult, op1=ALU.mult),
                          reads=[xt[b].tl(), rs[b].tl(), normt.tl()], writes=[xb[b].tl()])
                    for g in range(2):
                        pb = ptr[(2 * i + g) % 4]
                        for c in range(8):
                            cc = g * 8 + c
                            kb.op("pe", lambda e, b=b, pb=pb, c=c, cc=cc: e.transpose(
                                out=pb[:, c * 128:(c + 1) * 128], in_=xb[b][:, cc * 128:(cc + 1) * 128], identity=ident[:]),
                                reads=[xb[b].tl(), ident.tl()], writes=[pb.tl()])
                        dst = xnT[:, g * 8:(g + 1) * 8, i * 128:(i + 1) * 128]
                        src = pb[:].rearrange("p (c n) -> p c n", n=128)
                        if g == 0:
                            kb.op("act", lambda e, dst=dst, src=src: e.activation(out=dst, in_=src, func=AF.Copy),
                                  reads=[pb.tl()], writes=[xnT.tl(i)])
                        else:
                            kb.op("pool" if False else "dve", lambda e, dst=dst, src=src: e.tensor_copy(out=dst, in_=src),
                                  reads=[pb.tl()], writes=[xnT.tl(i)])
                kb.barrier()
                if debug == 10:
                    xnT_d = nc.dram_tensor("xnT_d", [128, 16, S], BF16, kind="ExternalOutput").ap()
                    s_dbg = kb.newsem("s_dbg")
                    kb.op("sp", lambda e: e.dma_start(out=xnT_d, in_=xnT[:]), dma_sem=s_dbg)
                    kb.barrier()
                    return nc
            with ExitStack() as sbk:
                NWB = 3
                wb = [sb(sbk, "wb%d" % i, [128, 16, 256], BF16) for i in range(NWB)]
                s_w = [kb.newsem("s_w") for _ in range(NWB)]
                stg = [sb(sbk, "stg%d" % i, [128, S], BF16) for i in range(2)]
                s_stg = [kb.newsem("s_stg") for _ in range(2)]
                stt = [sb(sbk, "stt%d" % i, [128, 8, 256], BF16) for i in range(2)]
                s_stt = [kb.newsem("s_stt") for _ in range(2)]
                pp = [ps(sbk, "pp%d" % i, [128, 512], F32) for i in range(6)]
                groups = []
                cidx = 0
                while cidx < C_GATE:
                    groups.append((cidx, 256))
                    cidx += 256
                groups.append((C_GATE, 24))
                cidx = C_NZ
                while cidx < NCOL:
                    groups.append((cidx, 256))
                    cidx += 256

                def col_kind(c0):
                    if C_SBV <= c0 < C_SBZ:
                        return ("tm", TM_SBV + c0 - C_SBV)
                    if C_VS <= c0 < C_KW:
                        return ("tm", TM_VS + c0 - C_VS)
                    if C_VW <= c0 < C_GATE:
                        return ("tm", TM_VW + c0 - C_VW)
                    if c0 < C_SBK or C_NQ <= c0 < C_KC:
                        return ("fm", "q")
                    if C_SBZ <= c0 < C_NQ or c0 >= C_NZ:
                        return ("fm", "silu")
                    if c0 == C_GATE:
                        return ("fm", "sig")
                    return ("fm", "copy")

                nfm = 0
                ntm = 0
                npp = 0
                import os
                if debug and os.environ.get("GSEL"):
                    groups = [groups[int(v)] for v in os.environ["GSEL"].split(",")]
                for gi, (c0, ncl) in enumerate(groups):
                    wbb = wb[gi % NWB]
                    kb.op("pool", lambda e, wbb=wbb, c0=c0, ncl=ncl: e.dma_start(
                        out=wbb[:, :, 0:ncl], in_=w_in[:, c0:c0 + ncl].rearrange("(c p) n -> p c n", p=128)),
                        writes=[wbb.tl()], dma_sem=s_w[gi % NWB])
                    kind, arg = col_kind(c0)
                    if kind == "fm":
                        for sgi in range((ncl + 127) // 128):
                            sc0 = sgi * 128
                            snc = min(128, ncl - sc0)
                            sg = stg[nfm % 2]
                            sgs = s_stg[nfm % 2]
                            nfm += 1
                            for qt in range(8):
                                pb = pp[npp % 6]
                                npp += 1
                                for kc in range(16):
                                    kb.op("pe", lambda e, pb=pb, wbb=wbb, kc=kc, sc0=sc0, snc=snc, qt=qt: e.matmul(
                                        pb[0:snc, :], lhsT=wbb[:, kc, sc0:sc0 + snc], rhs=xnT[:, kc, qt * 512:(qt + 1) * 512],
                                        start=(kc == 0), stop=(kc == 15)),
                                        reads=[wbb.tl()], writes=[pb.tl()])
                                dst = sg[0:snc, qt * 512:(qt + 1) * 512]
                                src = pb[0:snc, :]
                                if arg == "q":
                                    kb.op("act", lambda e, dst=dst, src=src: e.activation(out=dst, in_=src, func=AF.Copy, scale=SCALE),
                                          reads=[pb.tl()], writes=[sg.tl(qt)])
                                elif arg == "silu":
                                    kb.op("act", lambda e, dst=dst, src=src: e.activation(out=dst, in_=src, func=AF.Silu),
                                          reads=[pb.tl()], writes=[sg.tl(qt)])
                                elif arg == "sig":
                                    kb.op("act", lambda e, dst=dst, src=src: e.activation(out=dst, in_=src, func=AF.Sigmoid),
                                          reads=[pb.tl()], writes=[sg.tl(qt)])
                                else:
                                    kb.op("dve", lambda e, dst=dst, src=src: e.tensor_copy(out=dst, in_=src),
                                          reads=[pb.tl()], writes=[sg.tl(qt)])
                            kb.op("sp", lambda e, sg=sg, c0=c0, sc0=sc0, snc=snc: e.dma_start(
                                out=ft_d[c0 + sc0:c0 + sc0 + snc, :], in_=sg[0:snc, :]),
                                reads=[sg.tl(q) for q in range(8)], dma_sem=sgs)
                    else:
                        for i8 in range(4):
                            sg = stt[ntm % 2]
                            sgs = s_stt[ntm % 2]
                            ntm += 1
                            for ii in range(8):
                                i = i8 * 8 + ii
                                pb = pp[npp % 6]
                                npp += 1
                                for kc in range(16):
                                    kb.op("pe", lambda e, pb=pb, wbb=wbb, kc=kc, i=i, ncl=ncl: e.matmul(
                                        pb[:, 0:ncl], lhsT=xnT[:, kc, i * 128:(i + 1) * 128], rhs=wbb[:, kc, 0:ncl],
                                        start=(kc == 0), stop=(kc == 15)),
                                        reads=[wbb.tl()], writes=[pb.tl()])
                                dst = sg[:, ii, 0:ncl]
                                src = pb[:, 0:ncl]
                                if ii % 2 == 0:
                                    kb.op("dve", lambda e, dst=dst, src=src: e.tensor_copy(out=dst, in_=src),
                                          reads=[pb.tl()], writes=[sg.tl(ii)])
                                else:
                                    kb.op("act", lambda e, dst=dst, src=src: e.activation(out=dst, in_=src, func=AF.Copy),
                                          reads=[pb.tl()], writes=[sg.tl(ii)])
                            kb.op("sp", lambda e, sg=sg, arg=arg, i8=i8, ncl=ncl: e.dma_start(
                                out=tm_d[i8 * 1024:(i8 + 1) * 1024, arg:arg + ncl].rearrange("(i p) n -> p i n", p=128),
                                in_=sg[:, :, 0:ncl]),
                                reads=[sg.tl(q) for q in range(8)], dma_sem=sgs)
                kb.barrier()
        if debug == 1:
            return nc

        o_d = nc.dram_tensor("o_sc", [2048, S], BF16, kind=dk).ap()
        lv_d = nc.dram_tensor("lv_sc", [8, LV_LEN], BF16, kind=dk).ap()
        wv_d = nc.dram_tensor("wv_sc", [8, WV_LEN], BF16, kind=dk).ap()
        kcT = [sb(top, "kcT%d" % g, [128, 256], BF16) for g in range(2)]
        vcb = [sb(top, "vcb%d" % g, [128, 2, 128], BF16) for g in range(2)]

        def dma(eng, out, in_, sem, reads=(), writes=()):
            return kb.op(eng, lambda e: e.dma_start(out=out, in_=in_), reads=reads, writes=writes, dma_sem=sem)

        with ExitStack() as s2:
            relb = sb(s2, "relb", [33, 8], F32)
            ohl = sb(s2, "ohl", [33, LV_LEN], F32)
            ohw = sb(s2, "ohw", [33, WV_LEN], F32)
            lvs = sb(s2, "lvs", [8, LV_LEN], BF16)
            wvs = sb(s2, "wvs", [8, WV_LEN], BF16)
            pz = [ps(s2, "pz%d" % i, [128, 512], F32) for i in range(4)]
            sm = [kb.newsem("s2_%d" % i) for i in range(8)]
            kb.op("pool", lambda e: e.memset(relb[:], NEG), writes=[relb.tl()])
            dma("sp", relb[0:32, :], relb_d, sm[0], writes=[relb.tl()])
            dma("sp", ohl[:], ohl_d, sm[1], writes=[ohl.tl()])
            dma("sp", ohw[:], ohw_d, sm[2], writes=[ohw.tl()])
            nz = 0
            for (oh, n, dst, dd, sem) in ((ohl, LV_LEN, lvs, lv_d, sm[3]), (ohw, WV_LEN, wvs, wv_d, sm[4])):
                for c in range((n + 511) // 512):
                    w = min(512, n - c * 512)
                    pb = pz[nz % 4]
                    nz += 1
                    kb.op("pe", lambda e, pb=pb, oh=oh, c=c, w=w: e.matmul(pb[0:8, 0:w], lhsT=relb[:, :], rhs=oh[:, c * 512:c * 512 + w],
                                                                         start=True, stop=True),
                          reads=[relb.tl(), oh.tl()], writes=[pb.tl()])
                    kb.op("dve", lambda e, pb=pb, dst=dst, c=c, w=w: e.tensor_copy(out=dst[:, c * 512:c * 512 + w], in_=pb[0:8, 0:w]),
                          reads=[pb.tl()], writes=[dst.tl(c)])
                dma("sp", dd, dst[:], sem, reads=[dst.tl(c) for c in range((n + 511) // 512)])
            xcT = sb(s2, "xcT", [128, S], BF16)
            w1 = sb(s2, "w1", [128, 32, 256], BF16)
            w2 = sb(s2, "w2", [128, 2, 128], BF16)
            posf = sb(s2, "posf", [32, 128], F32)
            posT = sb(s2, "posT", [128, 32], BF16)
            hb = sb(s2, "hb", [128, 2], F32)
            hidT = sb(s2, "hidT", [128, 2, 256], BF16)
            g1 = sb(s2, "g1", [128, 256], F32)
            g2 = sb(s2, "g2", [128, 256], F32)
            g3 = sb(s2, "g3", [128, 256], F32)
            s_c = [kb.newsem("s2c_%d" % i) for i in range(4)]
            kb.op("pool", lambda e: e.memset(hidT[:], 0.0), writes=[hidT.tl(0), hidT.tl(1)])
            for which in range(2):
                w1_d, w2_d, pos_d, c_base = ((kw1_d, kw2_d, kpos_d, C_KC), (vw1_d, vw2_d, vpos_d, C_VC))[which]
                dma("pool", w1[:], w1_d.rearrange("l d e -> d l e"), s_c[0], writes=[w1.tl()])
                dma("pool", w2[:], w2_d.rearrange("(c p) f -> p c f", p=128), s_c[1], writes=[w2.tl()])
                dma("sp", posf[:], pos_d, s_c[2], writes=[posf.tl()])
                pb = pz[nz % 4]
                nz += 1
                kb.op("pe", lambda e, pb=pb: e.transpose(out=pb[:, 0:32], in_=posf[:, :], identity=identf[0:32, 0:32]),
                      reads=[posf.tl(), identf.tl()], writes=[pb.tl()])
                kb.op("dve", lambda e, pb=pb: e.tensor_copy(out=posT[:], in_=pb[:, 0:32]), reads=[pb.tl()], writes=[posT.tl()])
                pb = pz[nz % 4]
                nz += 1
                for eh in range(2):
                    for l in range(32):
                        kb.op("pe", lambda e, pb=pb, eh=eh, l=l: e.matmul(pb[:, eh:eh + 1], lhsT=w1[:, l, eh * 128:(eh + 1) * 128],
                                                                        rhs=posT[:, l:l + 1], start=(l == 0), stop=(l == 31)),
                              reads=[w1.tl(), posT.tl()], writes=[pb.tl()])
                kb.op("dve", lambda e, pb=pb: e.tensor_copy(out=hb[:], in_=pb[:, 0:2]), reads=[pb.tl()], writes=[hb.tl()])
                for g in range(2):
                    dma("sp", xcT[:], ft_d[c_base + g * 128:c_base + (g + 1) * 128, :], s_c[3], writes=[xcT.tl()])
                    for eh in range(2):
                        pb = pz[nz % 4]
                        nz += 1
                        for l in range(32):
                            kb.op("pe", lambda e, pb=pb, eh=eh, l=l: e.matmul(
                                pb[:, 0:255], lhsT=w1[:, l, eh * 128:(eh + 1) * 128],
                                rhs=xcT[:, l:l + 16 * 254 + 1:16], start=(l == 0), stop=(l == 31)),
                                reads=[w1.tl(), xcT.tl()], writes=[pb.tl()])
                        kb.op("act", lambda e, pb=pb, eh=eh: e.activation(out=g1[:, 0:255], in_=pb[:, 0:255], func=AF.Identity,
                                                                        bias=hb[:, eh:eh + 1]),
                              reads=[pb.tl(), hb.tl()], writes=[g1.tl()])
                        kb.op("dve", lambda e: e.tensor_tensor(out=g2[:, 0:255], in0=g1[:, 0:255], in1=g1[:, 0:255], op=ALU.mult),
                              reads=[g1.tl()], writes=[g2.tl()])
                        kb.op("dve", lambda e: e.tensor_scalar(out=g2[:, 0:255], in0=g2[:, 0:255], scalar1=0.044715, scalar2=1.0,
                                                               op0=ALU.mult, op1=ALU.add), reads=[g2.tl()], writes=[g2.tl()])
                        kb.op("dve", lambda e: e.tensor_tensor(out=g3[:, 0:255], in0=g2[:, 0:255], in1=g1[:, 0:255], op=ALU.mult),
                              reads=[g1.tl(), g2.tl()], writes=[g3.tl()])
                        kb.op("act", lambda e: e.activation(out=g3[:, 0:255], in_=g3[:, 0:255], func=AF.Sigmoid, scale=1.5957691216),
                              reads=[g3.tl()], writes=[g3.tl()])
                        kb.op("dve", lambda e, eh=eh: e.tensor_tensor(out=hidT[:, eh, 0:255], in0=g1[:, 0:255], in1=g3[:, 0:255], op=ALU.mult),
                              reads=[g1.tl(), g3.tl()], writes=[hidT.tl(eh)])
                    if which == 0:
                        pb = pz[nz % 4]
                        nz += 1
                        for eh in range(2):
                            kb.op("pe", lambda e, pb=pb, eh=eh: e.matmul(pb[:, 0:256], lhsT=w2[:, eh, :], rhs=hidT[:, eh, :],
                                                                       start=(eh == 0), stop=(eh == 1)),
                                  reads=[w2.tl(), hidT.tl(eh)], writes=[pb.tl()])
                        kb.op("dve", lambda e, pb=pb, g=g: e.tensor_copy(out=kcT[g][:], in_=pb[:, 0:256]), reads=[pb.tl()], writes=[kcT[g].tl()])
                    else:
                        for c in range(2):
                            pb = pz[nz % 4]
                            nz += 1
                            for eh in range(2):
                                kb.op("pe", lambda e, pb=pb, eh=eh, c=c: e.matmul(pb[:, 0:128], lhsT=hidT[:, eh, c * 128:(c + 1) * 128],
                                                                                rhs=w2[:, eh, :], start=(eh == 0), stop=(eh == 1)),
                                      reads=[w2.tl(), hidT.tl(eh)], writes=[pb.tl()])
                            kb.op("dve", lambda e, pb=pb, g=g, c=c: e.tensor_copy(out=vcb[g][:, c, :], in_=pb[:, 0:128]),
                                  reads=[pb.tl()], writes=[vcb[g].tl(c)])
            kb.barrier()
        if debug >= 2:
            dbg_d = nc.dram_tensor("dbg2", [128, 2, 256 + 256], BF16, kind="ExternalOutput").ap()
            sd = kb.newsem("sd")
            for g in range(2):
                dma("sp", dbg_d[:, g, 0:256], kcT[g][:], sd)
                dma("sp", dbg_d[:, g, 256:512], vcb[g][:].rearrange("p c f -> p (c f)"), sd)
            kb.barrier()
            if debug == 2:
                return nc

        def toep(tensor_ap, row, base, pstride, out_ap, sem, wt):
            src = bass.AP(tensor_ap.tensor, row * tensor_ap.shape[1] + base - 127 * pstride, [[pstride, 128], [1, 512]])
            return dma("sp", out_ap, src, sem, writes=wt)

        with ExitStack() as s3:
            PP = [ps(s3, "PP%d" % i, [128, 1024], F32) for i in range(4)]
            zb = [View(PP[0].t, 0, 512), View(PP[0].t, 512, 512), View(PP[1].t, 0, 512)]
            tbp = View(PP[1].t.bitcast(BF16), 1024, 1024)
            ub = View(PP[2].t, 0, 512)
            db = View(PP[2].t, 512, 512)
            mb = View(PP[3].t, 0, 512)
            gb = View(PP[3].t, 512, 512)
            nzb = [0]

            def nextz():
                b = zb[nzb[0] % 3]
                nzb[0] += 1
                return b

            with ExitStack() as ssb:
                qT = sb(ssb, "sb_qT", [128, S], BF16)
                kT = sb(ssb, "sb_kT", [128, S], BF16)
                vv = sb(ssb, "sb_v", [128, 32, 128], BF16)
                negm = sb(ssb, "negm", [128, 4, 512], BF16)
                ebuf = [sb(ssb, "ebuf%d" % i, [128, 1024], F32) for i in range(2)]
                spb = [sb(ssb, "spb%d" % i, [128, 1024], BF16) for i in range(4)]
                lsum = [sb(ssb, "lsum%d" % i, [128, 1024], BF16) for i in range(3)]
                aT = [sb(ssb, "aT%d" % i, [128, 1024], BF16) for i in range(3)]
                ost = [sb(ssb, "ost%d" % i, [128, 512], BF16) for i in range(2)]
                s_q, s_k, s_v = kb.newsem("sq"), kb.newsem("sk"), kb.newsem("sv")
                s_o = [kb.newsem("so") for _ in range(2)]
                kb.op("pool", lambda e: e.memset(negm[:], 0.0), writes=[negm.tl()])
                for di in range(4):
                    kb.op("pool", lambda e, di=di: e.affine_select(out=negm[:, di, :], in_=negm[:, di, :], pattern=[[1, 512]],
                                                                   compare_op=ALU.is_ge, fill=NEG, base=-128 * di - 1,
                                                                   channel_multiplier=-1),
                          reads=[negm.tl()], writes=[negm.tl()])
                ZZ = [View(PP[i].t, 0, 1024) for i in range(3)]
                ubs = [View(PP[3].t, 0, 512), View(PP[3].t, 512, 512)]
                nost = 0
                for h in range(8):
                    dma("sp", qT[:], ft_d[C_SBQ + h * 128:C_SBQ + (h + 1) * 128, :], s_q, writes=[qT.tl()])
                    dma("sp", kT[:], ft_d[C_SBK + h * 128:C_SBK + (h + 1) * 128, :], s_k, writes=[kT.tl()])
                    dma("sp", vv[:], tm_d[:, TM_SBV + h * 128:TM_SBV + (h + 1) * 128].rearrange("(i p) d -> p i d", p=128), s_v,
                        writes=[vv.tl()])
                    tasks = []
                    for qt in range(8):
                        kbs = list(range(4 * qt + 3, -1, -1))
                        for j in range(len(kbs) // 2):
                            tasks.append((qt, j, kbs[2 * j], kbs[2 * j + 1], len(kbs)))
                    nt = len(tasks)
                    st = {}

                    def stA(ti):
                        qt, j, kba, kbb, n = tasks[ti]
                        Z = ZZ[ti % 3]
                        for half, kbi in ((0, kba), (1, kbb)):
                            c0 = half * 512
                            kb.op("pe", lambda e, Z=Z, kbi=kbi, qt=qt, c0=c0: e.matmul(
                                Z[:, c0:c0 + 512], lhsT=kT[:, kbi * 128:(kbi + 1) * 128], rhs=qT[:, qt * 512:(qt + 1) * 512],
                                start=True, stop=False), reads=[kT.tl(), qT.tl()], writes=[Z.tl()])
                            if kbi >= 4 * qt:
                                di = kbi - 4 * qt
                                kb.op("pe", lambda e, Z=Z, di=di, c0=c0: e.matmul(Z[:, c0:c0 + 512], lhsT=ident[:, :], rhs=negm[:, di, :],
                                                                                start=False, stop=False),
                                      reads=[ident.tl(), negm.tl()], writes=[Z.tl()])
                        eb = ebuf[ti % 2]
                        sp = spb[ti % 4]
                        kb.op("act", lambda e, Z=Z, eb=eb: e.activation(out=eb[:], in_=Z[:, 0:1024], func=AF.Exp),
                              reads=[Z.tl()], writes=[eb.tl()])
                        kb.op("act", lambda e, eb=eb, sp=sp: e.activation(out=sp[:], in_=eb[:], func=AF.Ln, bias=1.0),
                              reads=[eb.tl()], writes=[sp.tl()])
                        ls = lsum[ti % 3]
                        if j == 0:
                            kb.op("pool", lambda e, sp=sp, ls=ls: e.tensor_copy(out=ls[:, 512:1024], in_=sp[:, 0:512]),
                                  reads=[sp.tl()], writes=[ls.tl()])
                        else:
                            psp, pls = st[ti - 1][1], st[ti - 1][2]
                            kb.op("pool", lambda e, psp=psp, pls=pls, ls=ls: e.tensor_tensor(
                                out=ls[:, 0:512], in0=pls[:, 512:1024], in1=psp[:, 512:1024], op=ALU.add),
                                reads=[psp.tl(), pls.tl()], writes=[ls.tl()])
                            kb.op("pool", lambda e, sp=sp, ls=ls: e.tensor_tensor(
                                out=ls[:, 512:1024], in0=ls[:, 0:512], in1=sp[:, 0:512], op=ALU.add),
                                reads=[sp.tl(), ls.tl()], writes=[ls.tl()])
                        st[ti] = (Z, sp, ls)

                    def stB1(ti):
                        qt, j, kba, kbb, n = tasks[ti]
                        Z, sp, ls = st[ti]
                        for half in (0, 1):
                            c0 = half * 512
                            idx = 2 * j + half
                            kb.op("pe", lambda e, Z=Z, sp=sp, c0=c0, idx=idx: e.matmul(Z[:, c0:c0 + 512], lhsT=negtri[:, :], rhs=sp[:, c0:c0 + 512],
                                                                                     start=False, stop=(idx == 0)),
                                  reads=[negtri.tl(), sp.tl()], writes=[Z.tl()])
                            if idx > 0:
                                kb.op("pe", lambda e, Z=Z, ls=ls, c0=c0: e.matmul(Z[:, c0:c0 + 512], lhsT=negones[:, :], rhs=ls[:, c0:c0 + 512],
                                                                                start=False, stop=True),
                                      reads=[negones.tl(), ls.tl()], writes=[Z.tl()])
                        a_ = aT[ti % 3]
                        kb.op("act", lambda e, Z=Z, a_=a_: e.activation(out=a_[:], in_=Z[:, 0:1024], func=AF.Exp),
                              reads=[Z.tl()], writes=[a_.tl()])

                    def stB2(ti):
                        nonlocal nost
                        qt, j, kba, kbb, n = tasks[ti]
                        a_ = aT[ti % 3]
                        u = ubs[qt % 2]
                        for half, kbi in ((0, kba), (1, kbb)):
                            c0 = half * 512
                            idx = 2 * j + half
                            kb.op("pe", lambda e, a_=a_, kbi=kbi, idx=idx, n=n, u=u, c0=c0: e.matmul(
                                u[:, :], lhsT=vv[:, kbi, :], rhs=a_[:, c0:c0 + 512], start=(idx == 0), stop=(idx == n - 1)),
                                reads=[vv.tl(), a_.tl()], writes=[u.tl()])
                        if 2 * j + 2 == n:
                            o = ost[nost % 2]
                            so = s_o[nost % 2]
                            nost += 1
                            kb.op("dve", lambda e, o=o, u=u: e.tensor_copy(out=o[:], in_=u[:, :]), reads=[u.tl()], writes=[o.tl()])
                            dma("sp", o_d[h * 128:(h + 1) * 128, qt * 512:(qt + 1) * 512], o[:], so, reads=[o.tl()])

                    for step in range(nt + 2):
                        if step < nt:
                            stA(step)
                        if 0 <= step - 1 < nt:
                            stB1(step - 1)
                        if 0 <= step - 2 < nt:
                            stB2(step - 2)
                kb.barrier()
            if debug == 3:
                return nc

            with ExitStack() as sn:
                qTn = [sb(sn, "n_qT%d" % r, [128, S], BF16) for r in range(4)]
                ocb = [sb(sn, "ocb%d" % r, [128, S], BF16) for r in range(4)]
                gateT = sb(sn, "gateT", [24, S], BF16)
                gsel = sb(sn, "gsel", [24, 24, 128], BF16)
                selT = sb(sn, "selT", [64, S], BF16)
                pT = [sb(sn, "pT%d" % i, [128, 512], BF16) for i in range(3)]
                rd = sb(sn, "rd", [128, 512], F32)
                ff = sb(sn, "ff", [128, 512], F32)
                tmpf = sb(sn, "tmpf", [128, 512], F32)
                oacc = sb(sn, "oacc", [128, 512], F32)
                ostn = [sb(sn, "ostn%d" % i, [128, 512], BF16) for i in range(2)]
                s_on = [kb.newsem("son") for _ in range(2)]
                s_ld = [kb.newsem("snl") for _ in range(12)]
                s_cb = [kb.newsem("scb") for _ in range(12)]
                s_sb = [kb.newsem("ssb") for _ in range(14)]
                s_wb = [kb.newsem("swb") for _ in range(8)]
                dma("sp", gateT[:], ft_d[C_GATE:C_GATE + 24, :], s_ld[0], writes=[gateT.tl()])
                dma("pool", gsel[:], gsel_d, s_ld[1], writes=[gsel.tl()])
                relb31 = sb(sn, "relb31", [128, 8], F32)
                dma("sp", relb31[:], relb_d[31:32, :].partition_broadcast(128), s_ld[11], writes=[relb31.tl()])
                npt = [0]
                nos = [0]

                def nextp():
                    p = pT[npt[0] % 3]
                    npt[0] += 1
                    return p

                def finalize(gidx, qt):
                    kb.op("act", lambda e: e.activation(out=rd[:], in_=db[:, :], func=AF.Ln, bias=1e-18),
                          reads=[db.tl()], writes=[rd.tl()])
                    kb.op("act", lambda e: e.activation(out=rd[:], in_=rd[:], func=AF.Exp, scale=-1.0), reads=[rd.tl()], writes=[rd.tl()])
                    kb.op("pe", lambda e: e.matmul(gb[:, :], lhsT=gsel[:, gidx, :], rhs=gateT[:, qt * 512:(qt + 1) * 512],
                                                   start=True, stop=True),
                          reads=[gsel.tl(), gateT.tl()], writes=[gb.tl()])
                    kb.op("dve", lambda e: e.tensor_tensor(out=ff[:], in0=rd[:], in1=gb[:, :], op=ALU.mult),
                          reads=[rd.tl(), gb.tl()], writes=[ff.tl()])

                for g in range(2):
                    for r in range(4):
                        h = g * 4 + r
                        dma("sp", qTn[r][:], ft_d[C_NQ + h * 128:C_NQ + (h + 1) * 128, :], s_ld[2 + r], writes=[qTn[r].tl()])
                    with ExitStack() as p1:
                        impT = sb(p1, "impT", [64, S], F32)
                        tka = sb(p1, "tka", [128, 32, 64], F32)
                        tkb = sb(p1, "tkb", [128, 32, 64], F32)
                        ovl = sb(p1, "ovl", [128, 2, 64], BF16)
                        cbias = sb(p1, "cbias", [128, 12, 512], BF16)
                        sc = sb(p1, "sc", [128, 64], F32)
                        sc2 = sb(p1, "sc2", [128, 64], F32)
                        m8a = sb(p1, "m8a", [128, 8], F32)
                        m8b = sb(p1, "m8b", [128, 8], F32)
                        snb = sb(p1, "snb", [128, 64], BF16)
                        dma("sp", tka[:], tka_d, s_ld[6], writes=[tka.tl()])
                        dma("sp", tkb[:], tkb_d, s_ld[7], writes=[tkb.tl()])
                        dma("pool", ovl[:], ovl_d, s_ld[8], writes=[ovl.tl()])
                        pairs = [(qt, 0) for qt in range(8)] + [(qt, 1) for qt in range(4, 8)]
                        pidx = {pr: i for i, pr in enumerate(pairs)}
                        for r in range(4):
                            h = g * 4 + r
                            for ci, (qt, c) in enumerate(pairs):
                                toep(lv_d, h, LV_OFF + 512 * qt - 2048 * c - 31, 16, cbias[:, ci, :], s_cb[ci], [cbias.tl(ci)])
                            for qt in range(8):
                                cs = [0] if qt < 4 else [0, 1]
                                for k_, c in enumerate(cs):
                                    z = nextz()
                                    ci = pidx[(qt, c)]
                                    kb.op("pe", lambda e, z=z, c=c, r=r, qt=qt: e.matmul(
                                        z[:, :], lhsT=kcT[g][:, c * 128:(c + 1) * 128], rhs=qTn[r][:, qt * 512:(qt + 1) * 512],
                                        start=True, stop=False), reads=[kcT[g].tl(), qTn[r].tl()], writes=[z.tl()])
                                    kb.op("pe", lambda e, z=z, ci=ci: e.matmul(z[:, :], lhsT=antiI[:, :], rhs=cbias[:, ci, :],
                                                                             start=False, stop=True),
                                          reads=[antiI.tl(), cbias.tl(ci)], writes=[z.tl()])
                                    p = nextp()
                                    kb.op("act", lambda e, z=z, p=p: e.activation(out=p[:], in_=z[:, :], func=AF.Exp),
                                          reads=[z.tl()], writes=[p.tl()])
                                    first, last = (k_ == 0), (k_ == len(cs) - 1)
                                    kb.op("pe", lambda e, p=p, c=c, first=first, last=last: e.matmul(
                                        ub[:, :], lhsT=vcb[g][:, c, :], rhs=p[:], start=first, stop=last),
                                        reads=[vcb[g].tl(c), p.tl()], writes=[ub.tl()])
                                    kb.op("pe", lambda e, p=p, first=first, last=last: e.matmul(
                                        db[:, :], lhsT=ones[:, :], rhs=p[:], start=first, stop=last),
                                        reads=[ones.tl(), p.tl()], writes=[db.tl()])
                                    kb.op("pe", lambda e, p=p, c=c, first=first, last=last: e.matmul(
                                        mb[0:64, :], lhsT=ovl[:, c, :], rhs=p[:], start=first, stop=last),
                                        reads=[ovl.tl(), p.tl()], writes=[mb.tl()])
                                finalize(g * 12 + r * 3 + 0, qt)
                                kb.op("dve", lambda e, r=r, qt=qt: e.tensor_tensor(out=ocb[r][:, qt * 512:(qt + 1) * 512], in0=ub[:, :],
                                                                                  in1=ff[:], op=ALU.mult),
                                      reads=[ub.tl(), ff.tl()], writes=[ocb[r].tl(qt)])
                                if r == 0:
                                    kb.op("dve", lambda e, qt=qt: e.tensor_tensor(out=impT[:, qt * 512:(qt + 1) * 512], in0=mb[0:64, :],
                                                                                 in1=rd[0:64, :], op=ALU.mult),
                                          reads=[mb.tl(), rd.tl()], writes=[impT.tl(qt)])
                                else:
                                    kb.op("dve", lambda e: e.tensor_tensor(out=tmpf[0:64, :], in0=mb[0:64, :], in1=rd[0:64, :], op=ALU.mult),
                                          reads=[mb.tl(), rd.tl()], writes=[tmpf.tl()])
                                    kb.op("dve", lambda e, qt=qt: e.tensor_tensor(out=impT[:, qt * 512:(qt + 1) * 512],
                                                                                 in0=impT[:, qt * 512:(qt + 1) * 512], in1=tmpf[0:64, :],
                                                                                 op=ALU.add),
                                          reads=[tmpf.tl(), impT.tl(qt)], writes=[impT.tl(qt)])
                        for i in range(32):
                            kb.op("pe", lambda e, i=i: e.transpose(out=gb[:, 0:64], in_=impT[:, i * 128:(i + 1) * 128],
                                                                   identity=identf[0:64, 0:64]),
                                  reads=[impT.tl(i // 4), identf.tl()], writes=[gb.tl()])
                            kb.op("dve", lambda e, i=i: e.tensor_tensor(out=sc[:], in0=gb[:, 0:64], in1=tka[:, i, :], op=ALU.mult),
                                  reads=[gb.tl(), tka.tl()], writes=[sc.tl()])
                            kb.op("dve", lambda e, i=i: e.tensor_tensor(out=sc[:], in0=sc[:], in1=tkb[:, i, :], op=ALU.add),
                                  reads=[sc.tl(), tkb.tl()], writes=[sc.tl()])
                            kb.op("dve", lambda e: e.max(out=m8a[:], in_=sc[:]), reads=[sc.tl()], writes=[m8a.tl()])
                            kb.op("dve", lambda e: e.match_replace(out=sc2[:], in_to_replace=m8a[:], in_values=sc[:], imm_value=-3e9),
                                  reads=[sc.tl(), m8a.tl()], writes=[sc2.tl()])
                            kb.op("dve", lambda e: e.max(out=m8b[:], in_=sc2[:]), reads=[sc2.tl()], writes=[m8b.tl()])
                            kb.op("dve", lambda e: e.tensor_scalar(out=snb[:], in0=sc[:], scalar1=m8b[:, 7:8], scalar2=-1.0,
                                                                   op0=ALU.is_ge, op1=ALU.add),
                                  reads=[sc.tl(), m8b.tl()], writes=[snb.tl()])
                            kb.op("pe", lambda e, i=i: e.transpose(out=tbp[0:64, (i % 8) * 128:(i % 8 + 1) * 128], in_=snb[:, :],
                                                                   identity=ident[:, :]),
                                  reads=[snb.tl(), ident.tl()], writes=[tbp.tl()])
                            if i % 8 == 7:
                                i8 = i // 8
                                kb.op("dve", lambda e, i8=i8: e.tensor_copy(out=selT[:, i8 * 1024:(i8 + 1) * 1024], in_=tbp[0:64, :]),
                                      reads=[tbp.tl()], writes=[selT.tl(2 * i8), selT.tl(2 * i8 + 1)])
                        kb.barrier()
                    with ExitStack() as p2:
                        ksT = sb(p2, "ksT", [128, S], BF16)
                        kwT = sb(p2, "kwT", [128, S], BF16)
                        vs = sb(p2, "vs", [128, 32, 128], BF16)
                        vw = sb(p2, "vw", [128, 32, 128], BF16)
                        x30 = sb(p2, "x30", [64, S], BF16)
                        sbias = sb(p2, "sbias", [128, 14, 512], BF16)
                        wbias = sb(p2, "wbias", [128, 8, 512], BF16)
                        dma("sp", ksT[:], ft_d[C_KS + g * 128:C_KS + (g + 1) * 128, :], s_ld[6], writes=[ksT.tl()])
                        dma("sp", kwT[:], ft_d[C_KW + g * 128:C_KW + (g + 1) * 128, :], s_ld[7], writes=[kwT.tl()])
                        dma("sp", vs[:], tm_d[:, TM_VS + g * 128:TM_VS + (g + 1) * 128].rearrange("(i p) d -> p i d", p=128), s_ld[8],
                            writes=[vs.tl()])
                        dma("sp", vw[:], tm_d[:, TM_VW + g * 128:TM_VW + (g + 1) * 128].rearrange("(i p) d -> p i d", p=128), s_ld[9],
                            writes=[vw.tl()])
                        dma("pool", x30[:], xexp_d, s_ld[10], writes=[x30.tl()])
                        for r in range(4):
                            h = g * 4 + r
                            for ti in range(14):
                                toep(lv_d, h, LV_OFF + (-384 + 128 * ti), 1, sbias[:, ti, :], s_sb[ti], [sbias.tl(ti)])
                            for ti in range(8):
                                toep(wv_d, h, WV_OFF + (-384 + 128 * ti), 1, wbias[:, ti, :], s_wb[ti], [wbias.tl(ti)])
                            tasks = []
                            for qt in range(8):
                                kbs = list(range(0, 4 * qt + 4))
                                for idx, kbi in enumerate(kbs):
                                    tasks.append(("s", qt, idx, kbi, len(kbs)))
                                kbs = list(range(max(0, 4 * qt - 4), 4 * qt + 4))
                                for idx, kbi in enumerate(kbs):
                                    tasks.append(("w", qt, idx, kbi, len(kbs)))
                            stt_ = {}

                            def front(ti, r=r, h=h):
                                far = False
                                kind, qt, idx, kbi, n = tasks[ti]
                                z = nextz()
                                delta = 512 * qt - 128 * kbi
                                if kind == "s":
                                    tix = min((delta + 384) // 128, 13)
                                    kb.op("pe", lambda e, z=z, kbi=kbi, qt=qt: e.matmul(
                                        z[:, :], lhsT=ksT[:, kbi * 128:(kbi + 1) * 128], rhs=qTn[r][:, qt * 512:(qt + 1) * 512],
                                        start=True, stop=False), reads=[ksT.tl(), qTn[r].tl()], writes=[z.tl()])
                                    far = delta >= 1280
                                    if not far:
                                        kb.op("pe", lambda e, z=z, tix=tix: e.matmul(z[:, :], lhsT=antiI[:, :], rhs=sbias[:, tix, :],
                                                                                   start=False, stop=False),
                                              reads=[antiI.tl(), sbias.tl(tix)], writes=[z.tl()])
                                    kb.op("pe", lambda e, z=z, kbi=kbi, qt=qt: e.matmul(
                                        z[:, :], lhsT=x30[:, kbi * 128:(kbi + 1) * 128], rhs=selT[:, qt * 512:(qt + 1) * 512],
                                        start=False, stop=True), reads=[x30.tl(), selT.tl(qt)], writes=[z.tl()])
                                else:
                                    tix = (delta + 384) // 128
                                    kb.op("pe", lambda e, z=z, kbi=kbi, qt=qt: e.matmul(
                                        z[:, :], lhsT=kwT[:, kbi * 128:(kbi + 1) * 128], rhs=qTn[r][:, qt * 512:(qt + 1) * 512],
                                        start=True, stop=False), reads=[kwT.tl(), qTn[r].tl()], writes=[z.tl()])
                                    kb.op("pe", lambda e, z=z, tix=tix: e.matmul(z[:, :], lhsT=antiI[:, :], rhs=wbias[:, tix, :],
                                                                               start=False, stop=True),
                                          reads=[antiI.tl(), wbias.tl(tix)], writes=[z.tl()])
                                p = nextp()
                                if kind == "s" and far:
                                    kb.op("act", lambda e, z=z, p=p: e.activation(out=p[:], in_=z[:, :], func=AF.Exp,
                                                                                 bias=relb31[:, h:h + 1]),
                                          reads=[z.tl(), relb31.tl()], writes=[p.tl()])
                                else:
                                    kb.op("act", lambda e, z=z, p=p: e.activation(out=p[:], in_=z[:, :], func=AF.Exp),
                                          reads=[z.tl()], writes=[p.tl()])
                                stt_[ti] = p

                            def back(ti, r=r, g=g, h=h):
                                kind, qt, idx, kbi, n = tasks[ti]
                                p = stt_.pop(ti)
                                first, last = (idx == 0), (idx == n - 1)
                                u = ub if kind == "s" else mb
                                vsrc = vs if kind == "s" else vw
                                kb.op("pe", lambda e, p=p, kbi=kbi, first=first, last=last, u=u, vsrc=vsrc: e.matmul(
                                    u[:, :], lhsT=vsrc[:, kbi, :], rhs=p[:], start=first, stop=last),
                                    reads=[vsrc.tl(), p.tl()], writes=[u.tl()])
                                kb.op("pe", lambda e, p=p, first=first, last=last: e.matmul(
                                    db[:, :], lhsT=ones[:, :], rhs=p[:], start=first, stop=last),
                                    reads=[ones.tl(), p.tl()], writes=[db.tl()])
                                if not last:
                                    return
                                if kind == "s":
                                    finalize(g * 12 + r * 3 + 1, qt)
                                    kb.op("dve", lambda e: e.tensor_tensor(out=oacc[:], in0=ub[:, :], in1=ff[:], op=ALU.mult),
                                          reads=[ub.tl(), ff.tl()], writes=[oacc.tl()])
                                    kb.op("dve", lambda e, qt=qt: e.tensor_tensor(out=oacc[:], in0=oacc[:],
                                                                                 in1=ocb[r][:, qt * 512:(qt + 1) * 512], op=ALU.add),
                                          reads=[oacc.tl(), ocb[r].tl(qt)], writes=[oacc.tl()])
                                else:
                                    finalize(g * 12 + r * 3 + 2, qt)
                                    kb.op("dve", lambda e: e.tensor_tensor(out=tmpf[:], in0=mb[:, :], in1=ff[:], op=ALU.mult),
                                          reads=[mb.tl(), ff.tl()], writes=[tmpf.tl()])
                                    o = ostn[nos[0] % 2]
                                    so = s_on[nos[0] % 2]
                                    nos[0] += 1
                                    kb.op("dve", lambda e, o=o: e.tensor_tensor(out=o[:], in0=oacc[:], in1=tmpf[:], op=ALU.add),
                                          reads=[oacc.tl(), tmpf.tl()], writes=[o.tl()])
                                    dma("sp", o_d[1024 + h * 128:1024 + (h + 1) * 128, qt * 512:(qt + 1) * 512], o[:], so, reads=[o.tl()])

                            nt = len(tasks)
                            front(0)
                            for ti in range(nt):
                                if ti + 1 < nt:
                                    front(ti + 1)
                                back(ti)
                        kb.barrier()
            if debug == 4:
                return nc

        with ExitStack() as s4:
            wo = sb(s4, "wo", [128, 16, D], BF16)
            nf = sb(s4, "nf", [128, D], F32)
            gn = sb(s4, "gn", [128, 16], F32)
            ot = sb(s4, "ot", [128, 16, 512], BF16)
            szt = sb(s4, "szt", [128, 16, 512], BF16)
            yTs = [sb(s4, "yT%d" % i, [128, 16, 512], BF16) for i in range(2)]
            sq = [sb(s4, "sq%d" % i, [128, 512], BF16) for i in range(2)]
            rstdb = [sb(s4, "rstdb%d" % i, [128, 512], F32) for i in range(2)]
            tmp4 = sb(s4, "tmp4", [128, 512], F32)
            xt2 = [sb(s4, "xt2_%d" % i, [128, D], F32) for i in range(2)]
            hb2 = sb(s4, "hb2", [128, D], F32)
            junk2 = sb(s4, "junk2", [128, D], F32)
            ob = sb(s4, "ob", [128, D], F32)
            ss2 = sb(s4, "ss2", [128, 1], F32)
            rs2 = sb(s4, "rs2", [128, 1], F32)
            pss = [ps(s4, "pss%d" % i, [128, 512], F32) for i in range(2)]
            ppo = [ps(s4, "ppo%d" % i, [128, 512], F32) for i in range(4)]
            s_w4 = [kb.newsem("sw4") for _ in range(4)]
            s_m = [kb.newsem("s4m") for _ in range(6)]
            s_x2 = [kb.newsem("sx2") for _ in range(2)]
            s_out = kb.newsem("sout")
            wo_v = w_out.rearrange("(c p) n -> p c n", p=128)
            for i in range(4):
                dma("pool", wo[:, 4 * i:4 * i + 4, :], wo_v[:, 4 * i:4 * i + 4, :], s_w4[i], writes=[wo.tl(i)])
            dma("sp", nf[:], normfin_d.partition_broadcast(128), s_m[0], writes=[nf.tl()])
            dma("sp", gn[:, 0:8], normsb_d, s_m[1], writes=[gn.tl(0)])
            dma("sp", gn[:, 8:16], normnsa_d, s_m[2], writes=[gn.tl(1)])
            npo = 0
            nx = 0

            def prep_parts(qt):
                yTq = yTs[qt % 2]

                def load():
                    dma("act", ot[:], o_d[:, qt * 512:(qt + 1) * 512].rearrange("(h p) t -> p h t", p=128), s_m[3], writes=[ot.tl()])
                    dma("act", szt[:, 0:8, :], ft_d[C_SBZ:C_SBZ + 1024, qt * 512:(qt + 1) * 512].rearrange("(h p) t -> p h t", p=128),
                        s_m[4], writes=[szt.tl(0)])
                    dma("act", szt[:, 8:16, :], ft_d[C_NZ:C_NZ + 1024, qt * 512:(qt + 1) * 512].rearrange("(h p) t -> p h t", p=128),
                        s_m[5], writes=[szt.tl(1)])

                def stat1(G, hh):
                    i = G * 8 + hh
                    q_ = sq[i % 2]
                    kb.op("act", lambda e, q_=q_, i=i: e.activation(out=q_[:], in_=ot[:, i, :], func=AF.Square),
                          reads=[ot.tl()], writes=[q_.tl()])
                    kb.op("pe", lambda e, q_=q_, G=G, hh=hh: e.matmul(pss[G][:, :], lhsT=ones[:, :], rhs=q_[:], start=(hh == 0), stop=(hh == 7)),
                          reads=[ones.tl(), q_.tl()], writes=[pss[G].tl()])

                def rstd(G):
                    rb_ = rstdb[G]
                    kb.op("dve", lambda e, rb_=rb_, G=G: e.tensor_scalar(out=rb_[:], in0=pss[G][:, :], scalar1=1.0 / 1024, scalar2=EPS,
                                                                         op0=ALU.mult, op1=ALU.add),
                          reads=[pss[G].tl()], writes=[rb_.tl()])
                    kb.op("act", lambda e, rb_=rb_: e.activation(out=rb_[:], in_=rb_[:], func=AF.Sqrt), reads=[rb_.tl()], writes=[rb_.tl()])
                    kb.op("dve", lambda e, rb_=rb_: e.reciprocal(out=rb_[:], in_=rb_[:]), reads=[rb_.tl()], writes=[rb_.tl()])

                def y1(G, hh):
                    rb_ = rstdb[G]
                    i = G * 8 + hh
                    kb.op("dve", lambda e, i=i, rb_=rb_: e.scalar_tensor_tensor(out=tmp4[:], in0=ot[:, i, :], scalar=gn[:, i:i + 1],
                                                                               in1=rb_[:], op0=ALU.mult, op1=ALU.mult),
                          reads=[ot.tl(), gn.tl(G), rb_.tl()], writes=[tmp4.tl()])
                    kb.op("dve", lambda e, i=i: e.tensor_tensor(out=yTq[:, i, :], in0=tmp4[:], in1=szt[:, i, :], op=ALU.mult),
                          reads=[tmp4.tl(), szt.tl(G)], writes=[yTq.tl(i)])

                th = [load]
                for G in range(2):
                    for hh in range(8):
                        th.append(lambda G=G, hh=hh: stat1(G, hh))
                    th.append(lambda G=G: rstd(G))
                for G in range(2):
                    for hh in range(8):
                        th.append(lambda G=G, hh=hh: y1(G, hh))
                return th

            def proj_tile(qt, ti):
                nonlocal npo, nx
                yTq = yTs[qt % 2]
                tok0 = qt * 512 + ti * 128
                xb_ = xt2[nx % 2]
                sx_ = s_x2[nx % 2]
                nx += 1
                dma("sp", xb_[:], x_d[tok0:tok0 + 128, :], sx_, writes=[xb_.tl()])
                for nb in range(4):
                    pb = ppo[npo % 4]
                    npo += 1
                    for f in range(16):
                        kb.op("pe", lambda e, pb=pb, f=f, nb=nb: e.matmul(
                            pb[:, :], lhsT=yTq[:, f, ti * 128:(ti + 1) * 128], rhs=wo[:, f, nb * 512:(nb + 1) * 512],
                            start=(f == 0), stop=(f == 15)), reads=[yTq.tl(f), wo.tl(f // 4)], writes=[pb.tl()])
                    kb.op("dve", lambda e, pb=pb, nb=nb, xb_=xb_: e.tensor_tensor(out=hb2[:, nb * 512:(nb + 1) * 512], in0=pb[:, :],
                                                                                 in1=xb_[:, nb * 512:(nb + 1) * 512], op=ALU.add),
                          reads=[pb.tl(), xb_.tl()], writes=[hb2.tl(nb)])
                    for _ in range(3):
                        if prepq:
                            prepq.pop(0)()
                hts = [hb2.tl(nb) for nb in range(4)]
                kb.op("act", lambda e: e.activation(out=junk2[:], in_=hb2[:], func=AF.Square, accum_out=ss2[:]),
                      reads=hts, writes=[junk2.tl(), ss2.tl()])
                kb.op("dve", lambda e: e.tensor_scalar(out=rs2[:], in0=ss2[:], scalar1=1.0 / D, scalar2=EPS, op0=ALU.mult, op1=ALU.add),
                      reads=[ss2.tl()], writes=[rs2.tl()])
                kb.op("act", lambda e: e.activation(out=rs2[:], in_=rs2[:], func=AF.Sqrt), reads=[rs2.tl()], writes=[rs2.tl()])
                kb.op("dve", lambda e: e.reciprocal(out=rs2[:], in_=rs2[:]), reads=[rs2.tl()], writes=[rs2.tl()])
                kb.op("act", lambda e: e.activation(out=junk2[:], in_=hb2[:], func=AF.Copy, scale=rs2[:, 0:1]),
                      reads=hts + [rs2.tl()], writes=[junk2.tl()])
                kb.op("pool", lambda e: e.tensor_tensor(out=ob[:], in0=junk2[:], in1=nf[:], op=ALU.mult),
                      reads=[junk2.tl(), nf.tl()], writes=[ob.tl()])
                dma("sp", out_d[tok0:tok0 + 128, :], ob[:], s_out, reads=[ob.tl()])

            prepq = []
            for f_ in prep_parts(0):
                f_()
            for qt in range(8):
                if qt + 1 < 8:
                    prepq.extend(prep_parts(qt + 1))
                    prepq.pop(0)()
                for ti in range(4):
                    proj_tile(qt, ti)
                while prepq:
                    prepq.pop(0)()
            kb.barrier()
    return nc


_NC_CACHE = {}


def _prep_inputs(inputs):
    c = _host_consts()
    f = lambda a: np.ascontiguousarray(np.asarray(a, dtype=np.float32))
    shared = {
        "w_in": f(inputs["w_in"][0]),
        "w_out": f(inputs["w_out"][0]),
        "norm_in": f(inputs["norm_in"][0]).reshape(1, D),
        "norm_sb": f(np.asarray(inputs["norm_sb"][0]).reshape(8, 128).T),
        "norm_nsa": f(np.asarray(inputs["norm_nsa"][0]).reshape(8, 128).T),
        "norm_final": f(inputs["norm_final"]).reshape(1, D),
        "rel_bias": f(inputs["rel_bias"]),
        "cmp_k_w1": f(inputs["cmp_k_w1"][0]),
        "cmp_k_w2": f(inputs["cmp_k_w2"][0]),
        "cmp_k_pos": f(inputs["cmp_k_pos"][0]),
        "cmp_v_w1": f(inputs["cmp_v_w1"][0]),
        "cmp_v_w2": f(inputs["cmp_v_w2"][0]),
        "cmp_v_pos": f(inputs["cmp_v_pos"][0]),
    }
    shared.update(c)
    return shared


def kernel(**inputs):
    shared = _prep_inputs(inputs)
    x = np.asarray(inputs["x"], dtype=np.float32)
    if "nc" not in _NC_CACHE:
        _NC_CACHE["nc"] = build(0)
    nc = _NC_CACHE["nc"]
    in_maps = []
    for b in range(8):
        m = dict(shared)
        m["x"] = np.ascontiguousarray(x[b])
        in_maps.append(m)
    res = run_bass_kernel_spmd(nc, in_maps, core_ids=list(range(8)))
    out = np.stack([np.asarray(res.results[b]["out"]) for b in range(8)], axis=0)
    return out.astype(np.float32)
```
